# Optimizing a Trainium2 kernel written in Bass

```python
import math
import jax, jax.numpy as jnp
from jax import lax
import numpy as np


D_MODEL = 2048
BATCH = 1
SEQ = 8192
DEPTH = 2

GRID_W = 64
CTX_LEN = 256
BLK = 128
SSD_HEADS = 16
SSD_HEADDIM = 64
SSD_INNER = SSD_HEADS * SSD_HEADDIM
SSD_STATE = 128
SSD_GROUPS = 2
SSD_CONV = 5
SSD_CHUNK = 128
SSD_XBC = SSD_INNER + 2 * SSD_GROUPS * SSD_STATE
HEAD_DIM = 64
WIN_QH = 8
WIN_KVH = 2
WINDOW = 128
ROPE_BASE = 10000.0
NAT_HEADS = 8
NAT_ROWS = 8
NAT_COLS = 16
MIX_WIDTH = SSD_INNER + WIN_QH * HEAD_DIM + NAT_HEADS * HEAD_DIM
IN_SIZES = (SSD_INNER, SSD_XBC, 2 * SSD_HEADS, WIN_QH * HEAD_DIM, WIN_KVH * HEAD_DIM, WIN_KVH * HEAD_DIM, NAT_HEADS * HEAD_DIM, NAT_HEADS * HEAD_DIM, NAT_HEADS * HEAD_DIM)
IN_WIDTH = sum(IN_SIZES)
PEER_HEADS = 8
PEER_KEYS = 128
PEER_EXPERTS = PEER_KEYS * PEER_KEYS
PEER_TOPK = 16
PEER_DK = 256
LN_EPS = 1e-6

kernel_name = 'hybrid_ssd_swa_natten_peer_diffusion_block'


def layer_norm(x, g, b):
    xf = x.astype(jnp.float32)
    mu = jnp.mean(xf, -1, keepdims=True)
    var = jnp.mean(jnp.square(xf - mu), -1, keepdims=True)
    return ((xf - mu) * lax.rsqrt(var + LN_EPS) * g.astype(jnp.float32) + b.astype(jnp.float32)).astype(x.dtype)


def rms_norm(x, g):
    xf = x.astype(jnp.float32)
    return (xf * lax.rsqrt(jnp.mean(jnp.square(xf), -1, keepdims=True) + LN_EPS) * g.astype(jnp.float32)).astype(x.dtype)


def depthwise_conv(u, w, b):
    k = w.shape[1]
    rhs = w.T[:, None, :]
    y = lax.conv_general_dilated(u, rhs, (1,), [(k // 2, k // 2)], dimension_numbers=('NWC', 'WIO', 'NWC'), feature_group_count=u.shape[-1])
    return y + b


def axial_rope(t, rows, cols):
    dh = t.shape[-1]
    half = dh // 2
    nf = half // 2
    inv = ROPE_BASE ** (-jnp.arange(nf, dtype=jnp.float32) / nf)

    def rot(part, pos):
        ang = pos.astype(jnp.float32)[:, None] * inv[None, :]
        cos = jnp.cos(ang)[None, :, None, :].astype(t.dtype)
        sin = jnp.sin(ang)[None, :, None, :].astype(t.dtype)
        p1, p2 = part[..., :nf], part[..., nf:]
        return jnp.concatenate([p1 * cos - p2 * sin, p1 * sin + p2 * cos], -1)

    return jnp.concatenate([rot(t[..., :half], rows), rot(t[..., half:], cols)], -1)


def ssd_chunked(xs, dt, a, bm, cm, h0, with_y):
    bsz, t_len, nh, hp = xs.shape
    ng, ns = bm.shape[2], bm.shape[3]
    q = SSD_CHUNK
    nc = t_len // q
    bh = jnp.repeat(bm.astype(jnp.float32), nh // ng, axis=2).reshape(bsz, nc, q, nh, ns)
    ch = jnp.repeat(cm.astype(jnp.float32), nh // ng, axis=2).reshape(bsz, nc, q, nh, ns)
    xdt = (xs.astype(jnp.float32) * dt[..., None]).reshape(bsz, nc, q, nh, hp)
    a_cs = jnp.cumsum((dt * a).reshape(bsz, nc, q, nh).transpose(0, 3, 1, 2), axis=-1)
    decay_to_end = jnp.exp(a_cs[..., -1:] - a_cs)
    states = jnp.einsum('bclhn,bhcl,bclhp->bchpn', bh, decay_to_end, xdt)
    chunk_decay = jnp.exp(a_cs[..., -1])

    def step(h, inp):
        dec, st = inp
        return dec[..., None, None] * h + st, h

    h_final, h_prev = lax.scan(step, h0, (chunk_decay.transpose(2, 0, 1), states.transpose(1, 0, 2, 3, 4)))
    if not with_y:
        return None, h_final
    h_prev = h_prev.transpose(1, 0, 2, 3, 4)
    idx = jnp.arange(q)
    lower = idx[:, None] >= idx[None, :]
    seg = a_cs[..., :, None] - a_cs[..., None, :]
    lmat = jnp.exp(jnp.where(lower, seg, -jnp.inf))
    y_diag = jnp.einsum('bclhn,bcshn,bhcls,bcshp->bclhp', ch, bh, lmat, xdt)
    y_off = jnp.einsum('bclhn,bchpn,bhcl->bclhp', ch, h_prev, jnp.exp(a_cs))
    return (y_diag + y_off).reshape(bsz, t_len, nh, hp), h_final


def ssd_mixer(z, xbc, dt, zc, xbcc, dtc, conv_w, conv_b, dt_bias, a_log, d_skip, norm_w, need_ctx):
    a = -jnp.exp(a_log.astype(jnp.float32))
    dtb = dt_bias.astype(jnp.float32).reshape(-1)
    flip = lambda t: jnp.flip(t, axis=1)

    def prep(u, dt_raw):
        u = jax.nn.silu(depthwise_conv(u, conv_w, conv_b))
        bsz, t_len = u.shape[0], u.shape[1]
        xs, bm, cm = jnp.split(u, [SSD_INNER, SSD_INNER + SSD_GROUPS * SSD_STATE], axis=-1)
        d = jax.nn.softplus(dt_raw.astype(jnp.float32) + dtb)
        return (xs.reshape(bsz, t_len, SSD_HEADS, SSD_HEADDIM), bm.reshape(bsz, t_len, SSD_GROUPS, SSD_STATE),
                cm.reshape(bsz, t_len, SSD_GROUPS, SSD_STATE), d[..., :SSD_HEADS], d[..., SSD_HEADS:])

    def bidir(xs, bm, cm, dtf, dtr, hf0, hb0, with_y):
        yf, hf = ssd_chunked(xs, dtf, a[0], bm, cm, hf0, with_y)
        yb, hb = ssd_chunked(flip(xs), flip(dtr), a[1], flip(bm), flip(cm), hb0, with_y)
        return yf, yb, hf, hb

    def finish(xs, yf, yb, zz):
        bsz, t_len = xs.shape[0], xs.shape[1]
        y = yf + flip(yb) + d_skip.astype(jnp.float32)[:, None] * xs.astype(jnp.float32)
        y = y.reshape(bsz, t_len, SSD_INNER).astype(zz.dtype)
        return rms_norm(y * jax.nn.silu(zz), norm_w)

    ctx_in = prep(xbcc, dtc)
    h0 = jnp.zeros((xbcc.shape[0], SSD_HEADS, SSD_HEADDIM, SSD_STATE), jnp.float32)
    ycf, ycb, hf, hb = bidir(*ctx_in, h0, h0, need_ctx)
    lat_in = prep(xbc, dt)
    yf, yb, _, _ = bidir(*lat_in, hf, hb, True)
    y = finish(lat_in[0], yf, yb, z)
    yc = finish(ctx_in[0], ycf, ycb, zc) if need_ctx else None
    return y, yc


def window_gqa(q, k, v, kc, vc, sink):
    bsz, t_len, hq, dh = q.shape
    hkv = k.shape[2]
    g = hq // hkv
    nb = t_len // BLK
    scale = dh ** -0.5
    qb = q.reshape(bsz, nb, BLK, hkv, g, dh)

    def bands(t):
        tp = jnp.pad(t, ((0, 0), (BLK, BLK), (0, 0), (0, 0))).reshape(bsz, nb + 2, BLK, hkv, dh)
        return jnp.concatenate([tp[:, :-2], tp[:, 1:-1], tp[:, 2:]], axis=2)

    kw, vw = bands(k), bands(v)
    qi = jnp.arange(BLK)[:, None]
    kj = jnp.arange(3 * BLK)[None, :] - BLK
    kabs = jnp.arange(nb)[:, None, None] * BLK + kj[None]
    mask = (jnp.abs(kj - qi) <= WINDOW)[None] & (kabs >= 0) & (kabs < t_len)
    s_loc = jnp.einsum('bnqhgd,bnkhd->bnhgqk', qb, kw).astype(jnp.float32) * scale
    s_loc = jnp.where(mask[None, :, None, None], s_loc, -jnp.inf)
    s_ctx = jnp.einsum('bnqhgd,bkhd->bnhgqk', qb, kc).astype(jnp.float32) * scale
    sink_col = jnp.broadcast_to(sink.astype(jnp.float32).reshape(1, 1, hkv, g, 1, 1), s_loc.shape[:-1] + (1,))
    p = jax.nn.softmax(jnp.concatenate([s_loc, s_ctx, sink_col], -1), axis=-1).astype(q.dtype)
    nloc = 3 * BLK
    nctx = kc.shape[1]
    o = jnp.einsum('bnhgqk,bnkhd->bnqhgd', p[..., :nloc], vw) + jnp.einsum('bnhgqk,bkhd->bnqhgd', p[..., nloc:nloc + nctx], vc)
    return o.reshape(bsz, t_len, hq * dh)


def ctx_attention(qc, kc, vc, sink):
    bsz, tc, hq, dh = qc.shape
    hkv = kc.shape[2]
    g = hq // hkv
    qg = qc.reshape(bsz, tc, hkv, g, dh)
    s = jnp.einsum('bqhgd,bkhd->bhgqk', qg, kc).astype(jnp.float32) * dh ** -0.5
    if sink is not None:
        s = jnp.concatenate([s, jnp.broadcast_to(sink.astype(jnp.float32).reshape(1, hkv, g, 1, 1), s.shape[:-1] + (1,))], -1)
    p = jax.nn.softmax(s, axis=-1)[..., :kc.shape[1]].astype(qc.dtype)
    return jnp.einsum('bhgqk,bkhd->bqhgd', p, vc).reshape(bsz, tc, hq * dh)


def neighbourhood_index(t_len):
    n_rows = t_len // GRID_W
    kr = min(NAT_ROWS, n_rows)
    t = jnp.arange(t_len)
    r = t // GRID_W
    col = t % GRID_W
    rs = jnp.clip(r - kr // 2, 0, n_rows - kr)
    cs = jnp.clip(col - NAT_COLS // 2, 0, GRID_W - NAT_COLS)
    key_r = rs[:, None, None] + jnp.arange(kr)[None, :, None]
    key_c = cs[:, None, None] + jnp.arange(NAT_COLS)[None, None, :]
    shape = (t_len, kr, NAT_COLS)
    idx = jnp.broadcast_to(key_r * GRID_W + key_c, shape).reshape(t_len, -1)
    dr = jnp.broadcast_to(key_r - r[:, None, None], shape).reshape(t_len, -1)
    dc = jnp.broadcast_to(key_c - col[:, None, None], shape).reshape(t_len, -1)
    return idx, dr, dc


def neighbourhood_attn(q, k, v, kc, vc, rpb):
    bsz, t_len, nh, dh = q.shape
    nb = t_len // BLK
    scale = dh ** -0.5
    idx, dr, dc = neighbourhood_index(t_len)
    nk = idx.shape[-1]

    def block(args):
        qb, ib, drb, dcb = args
        kg = k[:, ib]
        vg = v[:, ib]
        bias = rpb[:, drb + NAT_ROWS - 1, dcb + NAT_COLS - 1].astype(jnp.float32)
        s_n = jnp.einsum('bqhd,bqkhd->bhqk', qb, kg).astype(jnp.float32) * scale + bias[None]
        s_c = jnp.einsum('bqhd,bkhd->bhqk', qb, kc).astype(jnp.float32) * scale
        p = jax.nn.softmax(jnp.concatenate([s_n, s_c], -1), axis=-1).astype(qb.dtype)
        return jnp.einsum('bhqk,bqkhd->bqhd', p[..., :nk], vg) + jnp.einsum('bhqk,bkhd->bqhd', p[..., nk:], vc)

    qs = q.reshape(bsz, nb, BLK, nh, dh).transpose(1, 0, 2, 3, 4)
    o = lax.map(block, (qs, idx.reshape(nb, BLK, nk), dr.reshape(nb, BLK, nk), dc.reshape(nb, BLK, nk)))
    return o.transpose(1, 0, 2, 3, 4).reshape(bsz, t_len, nh * dh)


def mix_tokens(h, hc, rows, cols, w_in, conv_w, conv_b, dt_bias, a_log, d_skip, norm_w, sink, rpb, w_out, need_ctx):
    offs = np.cumsum(IN_SIZES)[:-1].tolist()
    z, xbc, dt, wq, wk, wv, nq, nk, nv = jnp.split(h @ w_in, offs, axis=-1)
    zc, xbcc, dtc, wqc, wkc, wvc, nqc, nkc, nvc = jnp.split(hc @ w_in, offs, axis=-1)
    heads = lambda t, n: t.reshape(t.shape[0], t.shape[1], n, HEAD_DIM)
    y_ssd, yc_ssd = ssd_mixer(z, xbc, dt, zc, xbcc, dtc, conv_w, conv_b, dt_bias, a_log, d_skip, norm_w, need_ctx)
    q = axial_rope(heads(wq, WIN_QH), rows, cols)
    k = axial_rope(heads(wk, WIN_KVH), rows, cols)
    v = heads(wv, WIN_KVH)
    kc_w, vc_w = heads(wkc, WIN_KVH), heads(wvc, WIN_KVH)
    y_win = window_gqa(q, k, v, kc_w, vc_w, sink)
    kc_n, vc_n = heads(nkc, NAT_HEADS), heads(nvc, NAT_HEADS)
    y_nat = neighbourhood_attn(heads(nq, NAT_HEADS), heads(nk, NAT_HEADS), heads(nv, NAT_HEADS), kc_n, vc_n, rpb)
    y = jnp.concatenate([y_ssd, y_win, y_nat], -1) @ w_out
    if not need_ctx:
        return y, None
    yc_win = ctx_attention(heads(wqc, WIN_QH), kc_w, vc_w, sink)
    yc_nat = ctx_attention(heads(nqc, NAT_HEADS), kc_n, vc_n, None)
    yc = jnp.concatenate([yc_ssd, yc_win, yc_nat], -1) @ w_out
    return y, yc


def peer_ffn(h, wq, k1, k2, u, v):
    bsz, t_len, d = h.shape
    half = PEER_DK // 2
    q = (h @ wq).reshape(bsz, t_len, PEER_HEADS, PEER_DK)
    s1 = jnp.einsum('bthd,nd->bthn', q[..., :half], k1).astype(jnp.float32)
    s2 = jnp.einsum('bthd,nd->bthn', q[..., half:], k2).astype(jnp.float32)
    v1, i1 = lax.top_k(s1, PEER_TOPK)
    v2, i2 = lax.top_k(s2, PEER_TOPK)
    cand = (v1[..., :, None] + v2[..., None, :]).reshape(bsz, t_len, PEER_HEADS, PEER_TOPK * PEER_TOPK)
    cidx = (i1[..., :, None] * PEER_KEYS + i2[..., None, :]).reshape(bsz, t_len, PEER_HEADS, PEER_TOPK * PEER_TOPK)
    top_s, pos = lax.top_k(cand, PEER_TOPK)
    eidx = jnp.take_along_axis(cidx, pos, axis=-1)
    gate = jax.nn.softmax(top_s, axis=-1).astype(h.dtype)
    nblk = bsz * t_len // BLK

    def block(args):
        hb, eb, gb = args
        act = jax.nn.gelu(jnp.einsum('thkd,td->thk', u[eb], hb), approximate=False)
        return jnp.einsum('thk,thkd->td', gb * act, v[eb])

    out = lax.map(block, (h.reshape(nblk, BLK, d), eidx.reshape(nblk, BLK, PEER_HEADS, PEER_TOPK), gate.reshape(nblk, BLK, PEER_HEADS, PEER_TOPK)))
    return out.reshape(bsz, t_len, d)


def setup_inputs(seed: int = 0) -> dict:
    key = jax.random.key(seed)
    ks = jax.random.split(key, 26)
    f32 = jnp.float32
    beta = (8.0 * DEPTH) ** -0.25
    nl = DEPTH

    def nrm(k, shape, s):
        return jax.random.normal(k, shape, f32) * s

    dt0 = jnp.exp(jax.random.uniform(ks[9], (nl, 2, SSD_HEADS), f32, math.log(1e-3), math.log(1e-1)))
    return {
        'x': nrm(ks[0], (BATCH, SEQ, D_MODEL), 1.0),
        'c': nrm(ks[1], (BATCH, D_MODEL), 1.0),
        'ctx': nrm(ks[2], (BATCH, CTX_LEN, D_MODEL), 1.0),
        'c_ctx': nrm(ks[3], (D_MODEL,), 1.0),
        'w_ada': nrm(ks[4], (nl, D_MODEL, 6 * D_MODEL), 0.5 * D_MODEL ** -0.5),
        'b_ada': nrm(ks[5], (nl, 6 * D_MODEL), 0.01),
        'w_in': nrm(ks[6], (nl, D_MODEL, IN_WIDTH), D_MODEL ** -0.5),
        'ssd_conv_w': nrm(ks[7], (nl, SSD_XBC, SSD_CONV), SSD_CONV ** -0.5),
        'ssd_conv_b': nrm(ks[8], (nl, SSD_XBC), 0.01),
        'ssd_dt_bias': dt0 + jnp.log(-jnp.expm1(-dt0)),
        'ssd_a_log': jnp.log(jax.random.uniform(ks[10], (nl, 2, SSD_HEADS), f32, 1.0, 16.0)),
        'ssd_d': 1.0 + nrm(ks[11], (nl, SSD_HEADS), 0.05),
        'ssd_norm_w': 1.0 + nrm(ks[12], (nl, SSD_INNER), 0.05),
        'attn_sink': nrm(ks[13], (nl, WIN_QH), 0.5),
        'nat_rpb': nrm(ks[14], (nl, NAT_HEADS, 2 * NAT_ROWS - 1, 2 * NAT_COLS - 1), 0.02),
        'w_out': nrm(ks[15], (nl, MIX_WIDTH, D_MODEL), beta * MIX_WIDTH ** -0.5),
        'ln1_g': 1.0 + nrm(ks[16], (nl, D_MODEL), 0.05),
        'ln1_b': nrm(ks[17], (nl, D_MODEL), 0.01),
        'peer_wq': nrm(ks[18], (nl, D_MODEL, PEER_HEADS * PEER_DK), D_MODEL ** -0.5),
        'peer_k1': nrm(ks[19], (nl, PEER_KEYS, PEER_DK // 2), (PEER_DK // 2) ** -0.5),
        'peer_k2': nrm(ks[20], (nl, PEER_KEYS, PEER_DK // 2), (PEER_DK // 2) ** -0.5),
        'peer_u': nrm(ks[21], (nl, PEER_EXPERTS, D_MODEL), D_MODEL ** -0.5),
        'peer_v': nrm(ks[22], (nl, PEER_EXPERTS, D_MODEL), beta),
        'ln2_g': 1.0 + nrm(ks[23], (nl, D_MODEL), 0.05),
        'ln2_b': nrm(ks[24], (nl, D_MODEL), 0.01),
    }


def reference(x, c, ctx, c_ctx, w_ada, b_ada, w_in, ssd_conv_w, ssd_conv_b, ssd_dt_bias, ssd_a_log, ssd_d, ssd_norm_w, attn_sink, nat_rpb, w_out, ln1_g, ln1_b, peer_wq, peer_k1, peer_k2, peer_u, peer_v, ln2_g, ln2_b):
    alpha = (2.0 * DEPTH) ** 0.25
    t_len = x.shape[1]
    pos = jnp.arange(t_len)
    rows, cols = pos // GRID_W, pos % GRID_W
    cx = ctx
    for l in range(DEPTH):
        need_ctx = l < DEPTH - 1
        mod = jax.nn.silu(c) @ w_ada[l] + b_ada[l]
        mod_c = jax.nn.silu(c_ctx) @ w_ada[l] + b_ada[l]
        sh1, sc1, g1, sh2, sc2, g2 = jnp.split(mod[:, None, :], 6, axis=-1)
        csh1, csc1, cg1, csh2, csc2, cg2 = jnp.split(mod_c, 6, axis=-1)
        h = x * (1.0 + sc1) + sh1
        hc = cx * (1.0 + csc1) + csh1
        y, yc = mix_tokens(h, hc, rows, cols, w_in[l], ssd_conv_w[l], ssd_conv_b[l], ssd_dt_bias[l], ssd_a_log[l], ssd_d[l], ssd_norm_w[l], attn_sink[l], nat_rpb[l], w_out[l], need_ctx)
        x = layer_norm(alpha * x + g1 * y, ln1_g[l], ln1_b[l])
        f = peer_ffn(x * (1.0 + sc2) + sh2, peer_wq[l], peer_k1[l], peer_k2[l], peer_u[l], peer_v[l])
        x = layer_norm(alpha * x + g2 * f, ln2_g[l], ln2_b[l])
        if need_ctx:
            cx = layer_norm(alpha * cx + cg1 * yc, ln1_g[l], ln1_b[l])
            fc = peer_ffn(cx * (1.0 + csc2) + csh2, peer_wq[l], peer_k1[l], peer_k2[l], peer_u[l], peer_v[l])
            cx = layer_norm(alpha * cx + cg2 * fc, ln2_g[l], ln2_b[l])
    return x
```

```python
import numpy as np
import concourse.bass as bass
import concourse.mybir as mybir
from concourse.bass_utils import run_bass_kernel_spmd

F32 = mybir.dt.float32
BF16 = mybir.dt.bfloat16
AF = mybir.ActivationFunctionType
ALU = mybir.AluOpType
AX = mybir.AxisListType


class Region:
    __slots__ = ("name", "last_w", "reads", "dsem", "dcount", "multi", "wlist")

    def __init__(self, name, multi=False):
        self.name = name
        self.last_w = None
        self.reads = []
        self.dsem = None
        self.dcount = 0
        self.multi = multi
        self.wlist = {}

    def writes(self):
        out = list(self.wlist.values())
        if self.last_w is not None:
            out.append(self.last_w)
        return out

    def note_write(self, tok):
        if self.multi:
            if tok[0] not in self.wlist or self.wlist[tok[0]][1] < tok[1]:
                self.wlist[tok[0]] = tok
        else:
            self.last_w = tok
            self.reads = []


class Prog:
    ENG = ("pe", "act", "dve", "pool", "sp")

    def __init__(self, nc):
        self.nc = nc
        self.q = {e: [] for e in self.ENG}
        self.cnt = {e: 0 for e in self.ENG}
        self.seen = {e: {} for e in self.ENG}
        self.sems = {}
        self.nsem = 0
        self.final_tokens = []
        self.dpool = {}
        self._pids = {}

    def _sem(self, key):
        if key not in self.sems:
            self.sems[key] = self.nc.alloc_semaphore("s%d" % self.nsem)
            self.nsem += 1
        return self.sems[key]

    def region(self, name):
        return Region(name)

    def _need(self, eng, tok, waits, same_ok=False):
        if tok is None:
            return
        key, val, teng = tok
        if same_ok and teng == eng:
            return
        if self.seen[eng].get(key, 0) >= val:
            return
        waits[key] = max(waits.get(key, 0), val)

    def op(self, eng, fn, reads=(), writes=(), accum=False, sreads=()):
        waits = {}
        for r in list(reads) + list(sreads):
            for t in r.writes():
                self._need(eng, t, waits)
        for w in writes:
            if accum and w.last_w is not None and w.last_w[2] == eng and not w.reads:
                continue
            self._need(eng, w.last_w, waits, same_ok=True)
            for t in w.reads:
                self._need(eng, t, waits, same_ok=True)
        for k, v in waits.items():
            self.seen[eng][k] = v
        self.cnt[eng] += 1
        tok = ("E" + eng, self.cnt[eng], eng)
        if eng == "pe":
            self.seen[eng][tok[0]] = tok[1]
        self.q[eng].append((list(waits.items()), fn, tok[0], 1))
        for r in list(reads) + list(sreads):
            r.reads.append(tok)
        for w in writes:
            w.last_w = tok
            w.reads = []
        return tok

    def dma(self, eng, fn, sb, reads=(), writes=()):
        waits = {}
        for r in reads:
            for t in r.writes():
                self._need(eng, t, waits)
        for w in writes:
            if not w.multi:
                self._need(eng, w.last_w, waits)
            for t in w.reads:
                self._need(eng, t, waits)
        for k, v in waits.items():
            self.seen[eng][k] = v
        pool = self.dpool.setdefault(eng, {"i": 0, "count": {}})
        npool = 12 if eng == "sp" else 8
        key = "D%s%d" % (eng, pool["i"] % npool)
        pool["i"] += 1
        self._sem(key)
        prev = pool["count"].get(key, 0)
        if prev > 0 and self.seen[eng].get(key, 0) < prev:
            waits[key] = prev
            self.seen[eng][key] = prev
        pool["count"][key] = prev + 16
        tok = (key, prev + 16, "dma")
        self.q[eng].append((list(waits.items()), fn, key, 16))
        for r in reads:
            r.reads.append(tok)
        for w in writes:
            w.note_write(tok)
        return tok

    def finish(self, tokens):
        self.final_tokens = list(tokens)

    def pid(self, engine):
        key = id(engine)
        if key not in self._pids:
            self._pids[key] = engine.partition_id()
        return self._pids[key]

    def emit(self):
        nc = self.nc
        handles = {"pe": "tensor", "act": "scalar", "dve": "vector", "pool": "gpsimd", "sp": "sync"}
        for e in self.ENG:
            self._sem("E" + e)
        with nc.Block() as blk:
            for e in self.ENG:
                entries = self.q[e]
                final = self.final_tokens if e == "sp" else []
                if not entries and not final:
                    continue

                def body(engine, entries=entries, final=final):
                    for waits, fn, skey, inc in entries:
                        for k, v in waits:
                            engine.wait_ge(self.sems[k], v)
                        ins = fn(engine)
                        ins.then_inc(self.sems[skey], inc)
                    for k, v, _ in final:
                        engine.wait_ge(self.sems[k], v)

                getattr(blk, handles[e])(body)


class Buf:
    def __init__(self, t, r):
        self.t, self.r = t, r

    def __getitem__(self, idx):
        return self.t[idx]


ARENA_WORDS = 52800
SHARED_INPUTS = ("cos", "sin", "wmask", "m01", "mneg", "ident", "tri", "iota")


class KB:
    def __init__(self, fused=False):
        self.nc = bass.Bass(target_bir_lowering=False) if fused else bass.Bass("TRN2", target_bir_lowering=False)
        self.P = Prog(self.nc)
        self.n = 0
        self.outs = []
        self.fused = fused
        self.pfx = ""
        self.io = {}
        self.ior = {}
        self.dins = {}
        self.arena = None
        if fused:
            nm = "arena"
            self.arena = Buf(self.nc.alloc_sbuf_tensor(nm, [128, ARENA_WORDS], F32), Region(nm))
            self.ps32 = [Buf(self.nc.alloc_psum_tensor("ps32_%d" % i, [128, 512], F32), Region("ps32_%d" % i)) for i in range(6)]
            self.ps16 = [Buf(self.nc.alloc_psum_tensor("ps16_%d" % i, [128, 1024], BF16), Region("ps16_%d" % i)) for i in range(2)]
            self.i32 = 0
            self.i16 = 0

    def phase_reset(self):
        self.reset(self.arena)

    def din(self, name, shape, dtype=F32):
        if name in self.io:
            return self.io[name]
        full = name if name in SHARED_INPUTS else self.pfx + name
        if full not in self.dins:
            self.dins[full] = self.nc.dram_tensor(full, list(shape), dtype, kind="ExternalInput").ap()
        return self.dins[full]

    def dout(self, name, shape, dtype=F32):
        if name in self.io:
            return self.io[name]
        return self.nc.dram_tensor(self.pfx + name, list(shape), dtype, kind="ExternalOutput").ap()

    def outr(self, name):
        return self.ior.get(name)

    def dscratch(self, name, shape, dtype=F32):
        return self.nc.dram_tensor(self.pfx + name, list(shape), dtype, kind="Internal").ap()

    def sb(self, shape, dtype=F32, name=None):
        if self.arena is not None:
            return self.carve(self.arena, shape, dtype)
        self.n += 1
        nm = "%s_%d" % (name or "sb", self.n)
        return Buf(self.nc.alloc_sbuf_tensor(nm, list(shape), dtype), Region(nm))

    def ps(self, shape=(128, 512), dtype=F32, name=None):
        if self.fused:
            if dtype == F32:
                b = self.ps32[self.i32 % 6]
                self.i32 += 1
            else:
                b = self.ps16[self.i16 % 2]
                self.i16 += 1
            return b
        self.n += 1
        nm = "%s_%d" % (name or "ps", self.n)
        return Buf(self.nc.alloc_psum_tensor(nm, list(shape), dtype), Region(nm))

    def carve(self, arena, shape, dtype=F32):
        off = getattr(arena, "_off", 0)
        n = int(np.prod(shape[1:]))
        nw = n if dtype in (F32, mybir.dt.uint32, mybir.dt.int32) else (n + 1) // 2
        assert off + nw <= arena.t.shape[1], "arena overflow %d + %d > %d" % (off, nw, arena.t.shape[1])
        arena._off = off + nw
        ap = arena.t[0:shape[0], off:off + nw]
        if dtype != F32:
            ap = ap.bitcast(dtype)
        if len(shape) == 3:
            ap = ap.rearrange("p (a b) -> p a b", b=shape[2])
        self.n += 1
        r = Region("cv%d" % self.n)
        r.reads = list(arena.r.reads) + ([arena.r.last_w] if arena.r.last_w is not None else [])
        b = Buf(ap, r)
        if not hasattr(arena, "_kids"):
            arena._kids = []
        arena._kids.append(b)
        return b

    def reset(self, arena):
        for kid in getattr(arena, "_kids", []):
            arena.r.reads += kid.r.reads + ([kid.r.last_w] if kid.r.last_w is not None else [])
        best = {}
        for t in arena.r.reads:
            if t[0] not in best or best[t[0]][1] < t[1]:
                best[t[0]] = t
        arena.r.reads = list(best.values())
        arena._kids = []
        arena._off = 0

    def load(self, dst_ap, dst_r, src_ap, q="sp", src_r=None):
        return self.P.dma(q, lambda e: e.dma_start(out=dst_ap, in_=src_ap), dst_r,
                          reads=[src_r] if src_r is not None else [], writes=[dst_r])

    def store(self, dst_ap, src_ap, src_r, q="sp", dst_r=None, slow=False):
        kw = {"allow_slow_non_contiguous": True} if slow else {}
        t = self.P.dma(q, lambda e: e.dma_start(out=dst_ap, in_=src_ap, **kw), src_r,
                       reads=[src_r], writes=[dst_r] if dst_r is not None else [])
        return t

    def mm(self, out_ap, out_r, lhsT, l_r, rhs, r_r, start=True, stop=True, extra_reads=()):
        return self.P.op("pe", lambda e: e.matmul(out_ap, lhsT=lhsT, rhs=rhs, start=start, stop=stop),
                         reads=[l_r, r_r] + list(extra_reads), writes=[out_r], accum=not start)

    def tr(self, out_ap, out_r, in_ap, in_r, ident_ap, ident_r, fresh=True):
        return self.P.op("pe", lambda e: e.transpose(out_ap, in_ap, ident_ap),
                         reads=[in_r, ident_r], writes=[out_r], accum=not fresh)

    def act(self, out_ap, out_r, in_ap, in_r, func, bias=None, scale=None, accum_out=None, reads=(), writes=()):
        kw = {}
        if bias is not None:
            kw["bias"] = bias
        if scale is not None:
            kw["scale"] = scale
        if accum_out is not None:
            kw["accum_out"] = accum_out
        return self.P.op("act", lambda e: e.activation(out=out_ap, in_=in_ap, func=func, **kw),
                         reads=[in_r], sreads=list(reads), writes=[out_r] + list(writes))

    def tt(self, out_ap, out_r, a_ap, a_r, b_ap, b_r, op, eng="dve"):
        return self.P.op(eng, lambda e: e.tensor_tensor(out=out_ap, in0=a_ap, in1=b_ap, op=op),
                         reads=[a_r, b_r], writes=[out_r])

    def ts(self, out_ap, out_r, a_ap, a_r, s1, op0, s2=None, op1=None, reads=(), eng="dve", accum_out=None, writes=()):
        kw = {}
        if accum_out is not None:
            kw["accum_out"] = accum_out
        if op1 is None:
            f = lambda e: e.tensor_scalar(out=out_ap, in0=a_ap, scalar1=s1, scalar2=None, op0=op0, **kw)
        else:
            f = lambda e: e.tensor_scalar(out=out_ap, in0=a_ap, scalar1=s1, scalar2=s2, op0=op0, op1=op1, **kw)
        return self.P.op(eng, f, reads=[a_r], sreads=list(reads), writes=[out_r] + list(writes))

    def stt(self, out_ap, out_r, a_ap, a_r, s, b_ap, b_r, op0, op1, reads=()):
        return self.P.op("dve", lambda e: e.scalar_tensor_tensor(out=out_ap, in0=a_ap, scalar=s, in1=b_ap, op0=op0, op1=op1),
                         reads=[a_r, b_r], sreads=list(reads), writes=[out_r])

    def copy(self, out_ap, out_r, in_ap, in_r, eng="dve"):
        if eng == "act":
            return self.P.op("act", lambda e: e.activation(out=out_ap, in_=in_ap, func=AF.Copy), reads=[in_r], writes=[out_r])
        return self.P.op(eng, lambda e: e.tensor_copy(out=out_ap, in_=in_ap), reads=[in_r], writes=[out_r])

    def memset(self, ap, r, val, eng="dve"):
        return self.P.op(eng, lambda e: e.memset(ap, val), writes=[r])

    def done(self, tokens):
        if self.fused:
            return tokens
        self.P.finish(tokens)
        self.P.emit()
        return self.nc

    def collective_allgather(self, src_ap, src_r, dst_ap, dst_r):
        P = self.P
        eng = "pool"
        waits = {}
        for t in src_r.writes():
            P._need(eng, t, waits)
        P._need(eng, dst_r.last_w, waits)
        for t in dst_r.reads:
            P._need(eng, t, waits)
        for kk, v in waits.items():
            P.seen[eng][kk] = v
        key = "C%d" % len(P.sems)
        P._sem(key)
        tok = (key, 1, "dma")
        groups = [list(range(NCORE))]
        fn = lambda e: e.collective_compute("AllGather", ALU.bypass, replica_groups=groups,
                                            ins=[src_ap.opt()], outs=[dst_ap.opt()])
        P.q[eng].append((list(waits.items()), fn, key, 1))
        src_r.reads.append(tok)
        dst_r.last_w = tok
        dst_r.reads = []
        return tok


class Ring:
    def __init__(self, items):
        self.items = items
        self.i = 0

    def next(self):
        it = self.items[self.i % len(self.items)]
        self.i += 1
        return it


def run_spmd(nc, in_maps):
    res = run_bass_kernel_spmd(nc, in_maps, core_ids=list(range(len(in_maps))))
    return res.results


D = 2048
SEQ = 8192
CTX = 256
NTOK = SEQ + CTX
DEPTH = 2
KC = D // 128
NCORE = 8
IN_SIZES = (1024, 1536, 32, 512, 128, 128, 512, 512, 512)
IN_OFF = np.concatenate([[0], np.cumsum(IN_SIZES)]).astype(int)
O_Z, O_XBC, O_DT, O_WQ, O_WK, O_WV, O_NQ, O_NK, O_NV = [int(v) for v in IN_OFF[:9]]
ALPHA = (2.0 * DEPTH) ** 0.25
LN_EPS = 1e-6
NEG = -30000.0


MODC = 12288 // NCORE


def build_mod(k=None, pfx=""):
    k = k or KB()
    k.pfx = pfx
    w = k.din("w", [DEPTH, D, MODC])
    b = k.din("b", [DEPTH, MODC])
    cv = k.din("cv", [128, 2, KC])
    out = k.dout("mod", [DEPTH, 2, MODC])
    cvs = k.sb([128, 2, KC])
    sv = k.sb([128, 2, KC])
    k.load(cvs[:], cvs.r, cv)
    k.act(sv[:], sv.r, cvs[:], cvs.r, AF.Silu)
    wb = Ring([k.sb([128, KC, 512], name="wb") for _ in range(2)])
    pb = Ring([k.ps() for _ in range(2)])
    bb = k.sb([2, DEPTH, MODC])
    for l in range(DEPTH):
        k.load(bb[:, l, :], bb.r, b[l, :].partition_broadcast(2))
    ob = k.sb([2, DEPTH, MODC])
    toks = []
    for l in range(DEPTH):
        for nb in range(MODC // 512):
            wt = wb.next()
            k.load(wt[:], wt.r, w[l, :, nb * 512:(nb + 1) * 512].rearrange("(c p) n -> p c n", p=128))
            p = pb.next()
            for c in range(KC):
                k.mm(p[0:2, :], p.r, sv[:, :, c], sv.r, wt[:, c, :], wt.r, start=(c == 0), stop=(c == KC - 1))
            k.tt(ob[:, l, nb * 512:(nb + 1) * 512], ob.r, p[0:2, :], p.r, bb[:, l, nb * 512:(nb + 1) * 512], bb.r, ALU.add)
        toks.append(k.store(out[l].rearrange("v n -> v n"), ob[:, l, :], ob.r, dst_r=k.outr("mod")))
    return k.done(toks)


def run_mod(c, c_ctx, w_ada, b_ada):
    nc = build_mod()
    cv = np.stack([c.reshape(D), c_ctx.reshape(D)], 0)
    cvl = np.ascontiguousarray(cv.reshape(2, KC, 128).transpose(2, 0, 1))
    maps = []
    for i in range(NCORE):
        maps.append({"w": np.ascontiguousarray(w_ada[:, :, i * MODC:(i + 1) * MODC]),
                     "b": np.ascontiguousarray(b_ada[:, i * MODC:(i + 1) * MODC]),
                     "cv": cvl})
    res = run_spmd(nc, maps)
    return np.concatenate([r["mod"] for r in res], axis=2)


def rope_tables():
    pos = np.arange(SEQ)
    rows, cols = (pos // 64).astype(np.float32), (pos % 64).astype(np.float32)
    inv = (10000.0 ** (-np.arange(16, dtype=np.float32) / 16)).astype(np.float32)
    cos = np.zeros((64, SEQ), np.float32)
    sin = np.zeros((64, SEQ), np.float32)
    for d in range(64):
        p = rows if d < 32 else cols
        ang = (p * inv[d % 16]).astype(np.float32)
        cos[d] = np.cos(ang)
        s = np.sin(ang)
        sin[d] = -s if (d % 32) < 16 else s
    return cos, sin


ROPE_PERM = np.array([d + 16 if (d % 32) < 16 else d - 16 for d in range(64)])

NAT_VARIANTS = [(10, 8, 5), (0, 0, 4), (1, 0, 4), (62, 60, 4), (63, 60, 4)]


def nat_variant(b):
    if b == 0:
        return 1
    if b == 1:
        return 2
    if b == 62:
        return 3
    if b == 63:
        return 4
    return 0


def nat_masks():
    m01 = np.zeros((5, 128, 640), np.float32)
    for v, (b, kb0, nkb) in enumerate(NAT_VARIANTS):
        q = np.arange(128)
        r = 2 * b + q // 64
        c = q % 64
        rs = np.clip(r - 4, 0, 120)
        cs = np.clip(c - 8, 0, 48)
        kk = np.arange(nkb * 128)
        kr = 2 * kb0 + kk // 64
        kc = kk % 64
        ok = (kr[None, :] >= rs[:, None]) & (kr[None, :] < rs[:, None] + 8) & \
             (kc[None, :] >= cs[:, None]) & (kc[None, :] < cs[:, None] + 16)
        m01[v, :, :nkb * 128] = ok
    mneg = (1.0 - m01) * NEG
    return m01, mneg.astype(np.float32)


def win_mask():
    i = np.arange(128)[:, None]
    jj = np.arange(384)[None, :]
    ok = (jj >= i) & (jj <= i + 256)
    return np.where(ok, 0.0, NEG).astype(np.float32)


def token_blocks():
    return [(0, CTX, True)] + [(CTX + 512 * j, 512, False) for j in range(SEQ // 512)]


def emit_modulate(k, xs, hT, n, scl, bia, pidx):
    for c in range(KC):
        s_ap = scl[:, pidx, c:c + 1]
        b_ap = bia[:, pidx, c:c + 1]
        if c % 2 == 0:
            k.act(hT[:, c, :n], hT.r, xs[:, c, :n], xs.r, AF.Identity, bias=b_ap, scale=s_ap, reads=[scl.r, bia.r])
        else:
            k.ts(hT[:, c, :n], hT.r, xs[:, c, :n], xs.r, s_ap, ALU.mult, b_ap, ALU.add, reads=[scl.r, bia.r])


def load_modp(k, modg, layer, mp, identf, psr):
    mg = modg.rearrange("(r l v n d) -> r l v n d", r=NCORE, l=DEPTH, v=2, n=6)
    m8 = k.sb([8, 4, 256])
    for vi, (v, n) in enumerate(((0, 1), (0, 0), (1, 1), (1, 0))):
        k.load(m8[:, vi, :], m8.r, mg[:, layer, v, n, :], src_r=k.outr("modg"))
    p = psr.next()
    for vi in range(4):
        for cc in range(2):
            j = vi * 2 + cc
            k.tr(p[:, j * 8:(j + 1) * 8], p.r, m8[:, vi, cc * 128:(cc + 1) * 128], m8.r, identf[0:8, 0:8], identf.r, fresh=(j == 0))
    k.copy(mp[:].rearrange("p v (r cc) -> p v cc r", cc=2), mp.r,
           p[:, 0:64].rearrange("p (v cc r) -> p v cc r", v=4, cc=2), p.r)


def load_h_block(k, x_src, xT, xs_flat, hT, g0, n, scl, bia, pidx, identf, psr, hsave=None, hload=None):
    if hload is not None:
        k.load(hT[:, :, :n], hT.r, hload[0][:, :, g0:g0 + n], src_r=hload[1])
        return
    if x_src is None:
        xs = Buf(xs_flat.t[:, :].rearrange("p (c t) -> p c t", t=512), xs_flat.r)
        k.load(xs[:, :, :n], xs.r, xT[:, g0:g0 + n].rearrange("(c p) t -> p c t", p=128))
        emit_modulate(k, xs, hT, n, scl, bia, pidx)
        return
    xt4 = Buf(xs_flat.t[:, :].rearrange("p (j d) -> p j d", d=D), xs_flat.r)
    nt = n // 128
    for j in range(nt):
        for (ap, p0, rows, sr) in x_src(g0 + 128 * j):
            k.load(xt4[p0:p0 + rows, j, :], xt4.r, ap, src_r=sr)
    for c in range(KC):
        p = psr.next()
        for j in range(nt):
            k.tr(p[:, j * 128:(j + 1) * 128], p.r, xt4[:, j, c * 128:(c + 1) * 128], xt4.r, identf[:], identf.r, fresh=(j == 0))
        s_ap = scl[:, pidx, c:c + 1]
        b_ap = bia[:, pidx, c:c + 1]
        if c % 2 == 0:
            k.act(hT[:, c, :n], hT.r, p[:, :n], p.r, AF.Identity, bias=b_ap, scale=s_ap, reads=[scl.r, bia.r])
        else:
            k.ts(hT[:, c, :n], hT.r, p[:, :n], p.r, s_ap, ALU.mult, b_ap, ALU.add, reads=[scl.r, bia.r])
    if hsave is not None:
        k.store(hsave[0][:, :, g0:g0 + n], hT[:, :, :n], hT.r, dst_r=hsave[1])


def build_attn(need_ctx, k=None, pfx="", x_src=None, modg=None, layer=0, hsave=None):
    k = k or KB()
    k.pfx = pfx
    xT = k.din("xT", [D, NTOK]) if x_src is None else None
    modp = k.din("modp", [128, 4, KC]) if modg is None else None
    wfm = k.din("wfm", [D, 384])
    wtm = k.din("wtm", [D, 128])
    cosd = k.din("cos", [64, SEQ])
    sind = k.din("sin", [64, SEQ])
    wmaskd = k.din("wmask", [128, 384])
    m01d = k.din("m01", [5, 128, 640])
    mnegd = k.din("mneg", [5, 128, 640])
    rpbd = k.din("rpb", [64, 15, 64])
    sinkd = k.din("sink", [1])
    identd = k.din("ident", [128, 128])
    out = k.dout("o", [NTOK, 128])

    psr = Ring([k.ps() for _ in range(6)])
    ptr = Ring([k.ps([128, 1024], BF16, "pt") for _ in range(2)])
    identf = None
    if x_src is not None:
        identf = k.sb([128, 128])
        k.load(identf[:], identf.r, identd)
    mp = k.sb([128, 4, KC])
    if modg is None:
        k.load(mp[:], mp.r, modp)
    else:
        load_modp(k, modg, layer, mp, identf, psr)
    scl = k.sb([128, 2, KC])
    bia = k.sb([128, 2, KC])
    k.ts(scl[:, 0, :], scl.r, mp[:, 0, :], mp.r, 1.0, ALU.add)
    k.ts(scl[:, 1, :], scl.r, mp[:, 2, :], mp.r, 1.0, ALU.add)
    k.copy(bia[:, 0, :], bia.r, mp[:, 1, :], mp.r)
    k.copy(bia[:, 1, :], bia.r, mp[:, 3, :], mp.r)
    wfb = k.sb([128, KC, 384], BF16)
    wtb = k.sb([128, KC, 128], BF16)
    k.load(wfb[:], wfb.r, wfm.rearrange("(c p) n -> p c n", p=128), q="pool")
    k.load(wtb[:], wtb.r, wtm.rearrange("(c p) n -> p c n", p=128), q="pool")
    ident = k.sb([128, 128], BF16)
    k.load(ident[:], ident.r, identd, q="pool")
    wmask = k.sb([128, 384])
    k.load(wmask[:], wmask.r, wmaskd)
    sink8 = k.sb([128, 1])
    k.load(sink8[:], sink8.r, sinkd[0:1].partition_broadcast(128))
    k.ts(sink8[:], sink8.r, sink8[:], sink8.r, 8.0, ALU.mult)
    bm = k.sb([128, 5, 640])
    raw = k.sb([128, 640])
    mt = k.sb([128, 640])
    for v, (b, kb0, nkb) in enumerate(NAT_VARIANTS):
        roff = 2 * kb0 - 2 * b
        for qr in range(2):
            d0 = roff - qr + 7
            k.load(raw[qr * 64:(qr + 1) * 64, 0:nkb * 128].rearrange("p (a b) -> p a b", b=64), raw.r,
                   rpbd[:, d0:d0 + 2 * nkb, :])
        k.load(mt[:], mt.r, m01d[v])
        k.ts(raw[:, 0:nkb * 128], raw.r, raw[:, 0:nkb * 128], raw.r, 8.0, ALU.mult)
        k.tt(bm[:, v, 0:nkb * 128], bm.r, raw[:, 0:nkb * 128], raw.r, mt[:, 0:nkb * 128], mt.r, ALU.mult)
        k.load(mt[:], mt.r, mnegd[v])
        k.tt(bm[:, v, 0:nkb * 128], bm.r, bm[:, v, 0:nkb * 128], bm.r, mt[:, 0:nkb * 128], mt.r, ALU.add)

    qT = k.sb([128, NTOK], BF16, "qT")
    kT = k.sb([128, NTOK], BF16, "kT")
    V = k.sb([128, NTOK // 128, 128], BF16, "V")
    xs_flat = k.sb([128, KC * 512], F32, "xs")
    hbs = Ring([k.sb([128, KC, 512], BF16, "hT") for _ in range(2)])
    cst = Ring([k.sb([64, 2, 512], F32, "cs") for _ in range(2)])
    tmpa = Ring([k.sb([64, 512], F32, "ta") for _ in range(2)])
    tmpb = Ring([k.sb([64, 512], F32, "tb") for _ in range(2)])

    for (g0, n, isctx) in token_blocks():
        hT = hbs.next()
        load_h_block(k, x_src, xT, xs_flat, hT, g0, n, scl, bia, 1 if isctx else 0, identf, psr, hsave=hsave)
        pss = {}
        for nm, c0, nc_ in (("Q", 0, 128), ("K", 128, 128), ("QP", 256, 64), ("KP", 320, 64)):
            if isctx and nm in ("QP", "KP"):
                continue
            p = psr.next()
            for c in range(KC):
                k.mm(p[0:nc_, :n], p.r, wfb[:, c, c0:c0 + nc_], wfb.r, hT[:, c, :n], hT.r, start=(c == 0), stop=(c == KC - 1))
            pss[nm] = p
        if isctx:
            k.copy(qT[:, g0:g0 + n], qT.r, pss["Q"][:, :n], pss["Q"].r, eng="act")
            k.copy(kT[:, g0:g0 + n], kT.r, pss["K"][:, :n], pss["K"].r, eng="act")
        else:
            cs = cst.next()
            k.load(cs[:, 0, :n], cs.r, cosd[:, g0 - CTX:g0 - CTX + n])
            k.load(cs[:, 1, :n], cs.r, sind[:, g0 - CTX:g0 - CTX + n])
            for nm, dst in (("Q", qT), ("K", kT)):
                ta, tb = tmpa.next(), tmpb.next()
                k.tt(ta[:, :n], ta.r, pss[nm][0:64, :n], pss[nm].r, cs[:, 0, :n], cs.r, ALU.mult)
                k.tt(tb[:, :n], tb.r, pss[nm + "P"][0:64, :n], pss[nm + "P"].r, cs[:, 1, :n], cs.r, ALU.mult)
                k.tt(dst[0:64, g0:g0 + n], dst.r, ta[:, :n], ta.r, tb[:, :n], tb.r, ALU.add, eng="pool")
                k.copy(dst[64:128, g0:g0 + n], dst.r, pss[nm][64:128, :n], pss[nm].r, eng="act")
        for tt_ in range(n // 128):
            p = psr.next()
            for c in range(KC):
                k.mm(p[:, 0:128], p.r, hT[:, c, tt_ * 128:(tt_ + 1) * 128], hT.r, wtb[:, c, :], wtb.r, start=(c == 0), stop=(c == KC - 1))
            k.copy(V[:, g0 // 128 + tt_, :], V.r, p[:, 0:128], p.r, eng=("act" if tt_ % 2 else "dve"))

    lbs = Ring([k.sb([128, 904], F32, "L") for _ in range(3)])
    pbs = Ring([k.sb([128, 904], BF16, "P") for _ in range(4)])
    pts = Ring([k.sb([128, 1024], BF16, "PT") for _ in range(2)])
    sml = Ring([k.sb([128, 4], F32, "sm") for _ in range(6)])
    obs = Ring([k.sb([128, 128], F32, "ob") for _ in range(4)])
    toks = []

    def att_front(q0, ht, k0, nloc, bias_ap, bias_r, use_sink, ob, store):
        pr = slice(0, 64) if ht == 0 else slice(64, 128)
        ncols = nloc + CTX + (1 if use_sink else 0)
        L = lbs.next()
        off = 0
        while off < nloc:
            kn = min(512, nloc - off)
            p = psr.next()
            k.mm(p[:, :kn], p.r, qT[pr, q0:q0 + 128], qT.r, kT[pr, k0 + off:k0 + off + kn], kT.r)
            k.tt(L[:, off:off + kn], L.r, p[:, :kn], p.r, bias_ap[:, off:off + kn], bias_r, ALU.add)
            off += kn
        p = psr.next()
        k.mm(p[:, :CTX], p.r, qT[pr, q0:q0 + 128], qT.r, kT[pr, 0:CTX], kT.r)
        k.copy(L[:, nloc:nloc + CTX], L.r, p[:, :CTX], p.r, eng="act")
        if use_sink:
            k.copy(L[:, nloc + CTX:nloc + CTX + 1], L.r, sink8[:], sink8.r, eng="pool")
        sm = sml.next()
        k.P.op("dve", lambda e: e.reduce_max(out=sm[:, 0:1], in_=L[:, :ncols], axis=AX.X), reads=[L.r], writes=[sm.r])
        k.ts(sm[:, 1:2], sm.r, sm[:, 0:1], sm.r, -0.125, ALU.mult)
        Pb = pbs.next()
        k.act(Pb[:, :ncols], Pb.r, L[:, :ncols], L.r, AF.Exp, bias=sm[:, 1:2], scale=0.125, accum_out=sm[:, 2:3],
              reads=[sm.r], writes=[sm.r])
        return (q0, ht, k0, nloc, sm, Pb, ob, store)

    def att_back(st):
        q0, ht, k0, nloc, sm, Pb, ob, store = st
        nkb = (nloc + CTX) // 128
        pt = ptr.next()
        for kb in range(nkb):
            k.tr(pt[:, kb * 128:(kb + 1) * 128], pt.r, Pb[:, kb * 128:(kb + 1) * 128], Pb.r, ident[:], ident.r, fresh=(kb == 0))
        PT = pts.next()
        k.copy(PT[:, :nkb * 128], PT.r, pt[:, :nkb * 128], pt.r, eng=("act" if ht else "dve"))
        po = psr.next()
        for kb in range(nkb):
            if kb * 128 < nloc:
                vt = (k0 + kb * 128) // 128
            else:
                vt = (kb * 128 - nloc) // 128
            k.mm(po[:, 0:64], po.r, PT[:, kb * 128:(kb + 1) * 128], PT.r, V[:, vt, ht * 64:(ht + 1) * 64], V.r,
                 start=(kb == 0), stop=(kb == nkb - 1))
        k.P.op("dve", lambda e: e.reciprocal(out=sm[:, 3:4], in_=sm[:, 2:3]), reads=[sm.r], writes=[sm.r])
        k.ts(ob[:, ht * 64:(ht + 1) * 64], ob.r, po[:, 0:64], po.r, sm[:, 3:4], ALU.mult, reads=[sm.r])
        if store is not None:
            toks.append(k.store(out[store:store + 128, :], ob[:], ob.r, dst_r=k.outr("o")))

    jobs = []
    if need_ctx:
        for qb in range(CTX // 128):
            ob = obs.next()
            jobs.append((qb * 128, 0, 0, 0, None, None, True, ob, None))
            jobs.append((qb * 128, 1, 0, 0, None, None, False, ob, qb * 128))
    NB = SEQ // 128
    for b in range(NB):
        ob = obs.next()
        q0 = CTX + b * 128
        lo, hi = max(b - 1, 0), min(b + 1, NB - 1)
        mo = (lo - (b - 1)) * 128
        jobs.append((q0, 0, CTX + lo * 128, (hi - lo + 1) * 128, wmask[:, mo:mo + (hi - lo + 1) * 128], wmask.r, True, ob, None))
        v = nat_variant(b)
        kb0 = b - 2 if v == 0 else NAT_VARIANTS[v][1]
        nkb = NAT_VARIANTS[v][2]
        jobs.append((q0, 1, CTX + kb0 * 128, nkb * 128, bm[:, v, 0:nkb * 128], bm.r, False, ob, q0))
    ALAG = 2
    pend = []
    for jb in jobs:
        pend.append(att_front(*jb))
        if len(pend) > ALAG:
            att_back(pend.pop(0))
    while pend:
        att_back(pend.pop(0))
    return k.done(toks)


def attn_inputs(l, i, xT, modp, w_in, attn_sink, nat_rpb, consts):
    kv = i // 4
    w = w_in[l]
    wq = w[:, O_WQ + 64 * i:O_WQ + 64 * (i + 1)]
    wk = w[:, O_WK + 64 * kv:O_WK + 64 * (kv + 1)]
    nq = w[:, O_NQ + 64 * i:O_NQ + 64 * (i + 1)]
    nk = w[:, O_NK + 64 * i:O_NK + 64 * (i + 1)]
    wv = w[:, O_WV + 64 * kv:O_WV + 64 * (kv + 1)]
    nv = w[:, O_NV + 64 * i:O_NV + 64 * (i + 1)]
    wfm = np.ascontiguousarray(np.concatenate([wq, nq, wk, nk, wq[:, ROPE_PERM], wk[:, ROPE_PERM]], axis=1))
    wtm = np.ascontiguousarray(np.concatenate([wv, nv], axis=1))
    jj = np.arange(64)[None, :] - np.arange(64)[:, None] + 15
    okj = (jj >= 0) & (jj <= 30)
    g = nat_rpb[l, i][:, np.clip(jj, 0, 30)]
    rpb = np.ascontiguousarray(np.where(okj[None], g, 0.0).transpose(1, 0, 2).astype(np.float32))
    d = {"xT": xT, "modp": modp, "wfm": wfm, "wtm": wtm, "rpb": rpb,
         "sink": np.ascontiguousarray(attn_sink[l, i:i + 1])}
    d.update(consts)
    return d


def attn_consts():
    cos, sin = rope_tables()
    m01, mneg = nat_masks()
    return {"cos": cos, "sin": sin, "wmask": win_mask(), "m01": m01, "mneg": mneg,
            "ident": np.eye(128, dtype=np.float32)}


def mod_pack(mod_l):
    sh1, sc1 = mod_l[0, 0:D], mod_l[0, D:2 * D]
    csh1, csc1 = mod_l[1, 0:D], mod_l[1, D:2 * D]
    a = np.stack([sc1, sh1, csc1, csh1], 0)
    return np.ascontiguousarray(a.reshape(4, KC, 128).transpose(2, 0, 1))


def build_ssd(need_ctx, debug=False, k=None, pfx="", x_src=None, modg=None, layer=0, hload=None):
    k = k or KB()
    k.pfx = pfx
    xT = k.din("xT", [D, NTOK]) if x_src is None else None
    modp = k.din("modp", [128, 4, KC]) if modg is None else None
    wfm = k.din("wfm", [D, 384])
    wtm = k.din("wtm", [D, 132])
    cwd = k.din("cw", [128, 3, 5])
    cbd = k.din("cb", [128, 3])
    dtbd = k.din("dtb", [4])
    alogd = k.din("alog", [4])
    dskd = k.din("dsk", [2])
    identd = k.din("ident", [128, 128])
    trid = k.din("tri", [2, 128, 128])
    outv = k.dout("v", [NTOK, 128])
    outs = k.dout("ssq", [NTOK, 1])
    NCH = NTOK // 128

    psr = Ring([k.ps() for _ in range(6)])
    ptr = Ring([k.ps([128, 1024], BF16, "pt") for _ in range(2)])
    identf = None
    scl = bia = None
    if hload is None:
        if x_src is not None:
            identf = k.sb([128, 128])
            k.load(identf[:], identf.r, identd)
        mp = k.sb([128, 4, KC])
        if modg is None:
            k.load(mp[:], mp.r, modp)
        else:
            load_modp(k, modg, layer, mp, identf, psr)
        scl = k.sb([128, 2, KC])
        bia = k.sb([128, 2, KC])
        k.ts(scl[:, 0, :], scl.r, mp[:, 0, :], mp.r, 1.0, ALU.add)
        k.ts(scl[:, 1, :], scl.r, mp[:, 2, :], mp.r, 1.0, ALU.add)
        k.copy(bia[:, 0, :], bia.r, mp[:, 1, :], mp.r)
        k.copy(bia[:, 1, :], bia.r, mp[:, 3, :], mp.r)
    wfb = k.sb([128, KC, 384], BF16)
    wtb = k.sb([128, KC, 132], BF16)
    k.load(wfb[:], wfb.r, wfm.rearrange("(c p) n -> p c n", p=128), q="pool")
    k.load(wtb[:], wtb.r, wtm.rearrange("(c p) n -> p c n", p=128), q="pool")
    ident = k.sb([128, 128], BF16)
    k.load(ident[:], ident.r, identd, q="pool")
    tri = k.sb([128, 2, 128])
    k.load(tri[:], tri.r, trid.rearrange("a p n -> p a n"))
    cw = k.sb([128, 3, 5])
    cb = k.sb([128, 3])
    k.load(cw[:], cw.r, cwd)
    k.load(cb[:], cb.r, cbd)
    dtb = k.sb([128, 4])
    aneg = k.sb([128, 4])
    dsk = k.sb([128, 2])
    k.load(dtb[:], dtb.r, dtbd.partition_broadcast(128))
    k.load(aneg[:], aneg.r, alogd.partition_broadcast(128))
    k.load(dsk[:], dsk.r, dskd.partition_broadcast(128))
    k.act(aneg[:], aneg.r, aneg[:], aneg.r, AF.Exp)
    k.ts(aneg[:], aneg.r, aneg[:], aneg.r, -1.0, ALU.mult)

    fm = [k.sb([128, NTOK], BF16, nm) for nm in ("xc", "Bc", "Cc")]
    zt = k.sb([128, NCH, 128], F32, "z")
    dtr = k.sb([128, NCH, 4], F32, "dtr")
    yacc = k.sb([128, NCH, 128], F32, "yacc")
    arena = k.sb([128, KC * 512], F32, "arena")
    hbs = Ring([k.sb([128, KC, 512], BF16, "hT")])
    stg = k.sb([128, 3, 520], F32, "stg")
    accs = Ring([k.sb([128, 512], F32, "acc") for _ in range(2)])

    def conv_emit(ch, i0, W, tok0):
        acc = accs.next()
        k.ts(acc[:, :W], acc.r, stg[:, ch, i0:i0 + W], stg.r, cw[:, ch, 0:1], ALU.mult, cb[:, ch:ch + 1], ALU.add,
             reads=[cw.r, cb.r])
        for j in range(1, 5):
            k.stt(acc[:, :W], acc.r, stg[:, ch, i0 + j:i0 + j + W], stg.r, cw[:, ch, j:j + 1], acc[:, :W], acc.r,
                  ALU.mult, ALU.add, reads=[cw.r])
        k.act(fm[ch][:, tok0:tok0 + W], fm[ch].r, acc[:, :W], acc.r, AF.Silu)

    blocks = token_blocks()
    for bi, (g0, n, isctx) in enumerate(blocks):
        first = (g0 == 0) or (g0 == CTX)
        last = (g0 + n == CTX) or (g0 + n == NTOK)
        hT = hbs.next()
        load_h_block(k, x_src, xT, arena, hT, g0, n, scl, bia, 1 if isctx else 0, identf, psr, hload=hload)
        if first:
            k.memset(stg[:, :, 0:4], stg.r, 0.0)
        for ch in range(3):
            p = psr.next()
            for c in range(KC):
                k.mm(p[:, :n], p.r, wfb[:, c, ch * 128:(ch + 1) * 128], wfb.r, hT[:, c, :n], hT.r,
                     start=(c == 0), stop=(c == KC - 1))
            k.copy(stg[:, ch, 4:4 + n], stg.r, p[:, :n], p.r, eng="act")
        i0 = 2 if first else 0
        for ch in range(3):
            conv_emit(ch, i0, n - i0, g0 - 2 + i0)
        k.copy(stg[:, :, 0:4], stg.r, stg[:, :, n:n + 4], stg.r, eng="pool")
        if last:
            k.memset(stg[:, :, 4:6], stg.r, 0.0)
            for ch in range(3):
                conv_emit(ch, 0, 2, g0 + n - 2)
        for tt_ in range(n // 128):
            p = psr.next()
            for c in range(KC):
                k.mm(p[:, 0:132], p.r, hT[:, c, tt_ * 128:(tt_ + 1) * 128], hT.r, wtb[:, c, :], wtb.r,
                     start=(c == 0), stop=(c == KC - 1))
            ci = g0 // 128 + tt_
            k.act(zt[:, ci, :], zt.r, p[:, 0:128], p.r, AF.Silu)
            k.copy(dtr[:, ci, :], dtr.r, p[:, 128:132], p.r, eng="dve")

    dtv = k.sb([128, NCH, 4], F32, "dt")
    dA = k.sb([128, NCH, 4], F32, "dA")
    t1 = k.sb([128, NCH, 4], F32, "t1")
    k.tt(dtr[:], dtr.r, dtr[:], dtr.r, dtb[:].unsqueeze(1).broadcast_to([128, NCH, 4]), dtb.r, ALU.add)
    k.act(t1[:], t1.r, dtr[:], dtr.r, AF.Abs)
    k.act(t1[:], t1.r, t1[:], t1.r, AF.Exp, scale=-1.0)
    k.act(t1[:], t1.r, t1[:], t1.r, AF.Ln, bias=1.0)
    k.ts(dtv[:], dtv.r, dtr[:], dtr.r, 0.0, ALU.max)
    k.tt(dtv[:], dtv.r, dtv[:], dtv.r, t1[:], t1.r, ALU.add)
    k.tt(dA[:], dA.r, dtv[:], dtv.r, aneg[:].unsqueeze(1).broadcast_to([128, NCH, 4]), aneg.r, ALU.mult)

    ACOL = k.sb([128, NCH, 4], F32, "ACOL")
    WDEC = k.sb([128, NCH, 4], F32, "WDEC")
    CDEC = k.sb([128, NCH, 4], F32, "CDEC")
    DTW = k.sb([128, NCH, 4], F32, "DTW")
    ones_f = k.sb([128, 128], F32, "ones")
    k.memset(ones_f[:], ones_f.r, 1.0)
    for dr_ in range(2):
        pq = psr.next()
        k.mm(pq[:, 0:NCH * 2].rearrange("p (c j) -> p c j", j=2), pq.r, tri[:, dr_, :], tri.r, dA[:, :, dr_ * 2:dr_ * 2 + 2], dA.r)
        k.copy(ACOL[:, :, dr_ * 2:dr_ * 2 + 2], ACOL.r, pq[:, 0:NCH * 2].rearrange("p (c j) -> p c j", j=2), pq.r, eng="act")
    pq = psr.next()
    k.mm(pq[:, 0:NCH * 4], pq.r, ones_f[:], ones_f.r, dA[:].rearrange("p c j -> p (c j)"), dA.r)
    k.tt(WDEC[:].rearrange("p c j -> p (c j)"), WDEC.r, pq[:, 0:NCH * 4], pq.r, ACOL[:].rearrange("p c j -> p (c j)"), ACOL.r, ALU.subtract)
    k.act(WDEC[:], WDEC.r, WDEC[:], WDEC.r, AF.Exp)
    k.act(CDEC[:].rearrange("p c j -> p (c j)"), CDEC.r, pq[:, 0:NCH * 4], pq.r, AF.Exp)
    k.tt(DTW[:], DTW.r, dtv[:], dtv.r, WDEC[:], WDEC.r, ALU.mult)

    H = [k.sb([128, 64], F32, "H%d" % c) for c in range(4)]
    Hb = [k.sb([128, 64], BF16, "Hb%d" % c) for c in range(4)]
    for c in range(4):
        k.memset(H[c][:], H[c].r, 0.0)
        k.memset(Hb[c][:], Hb[c].r, 0.0, eng="pool")

    def ring(shape, dt_, nm, n=2):
        return Ring([k.carve(arena, shape, dt_) for _ in range(n)])
    xtk_r, btk_r = ring([128, 128], BF16, "xtk", 5), ring([128, 128], BF16, "btk", 5)
    cbm_r = ring([128, 128], F32, "cbm", 3)
    acol_r = ring([128, 2], F32, "acol", 5)
    dab_r = ring([128, 128], F32, "dab", 4)
    sm_r = ring([128, 4], F32, "sm", 10)
    d_r = ring([128, 128], F32, "dd", 4)
    mt_r = ring([128, 128], BF16, "mt", 10)
    e2_r = ring([128, 128], F32, "e2", 4)
    ce_r = ring([128, 128], BF16, "ce", 10)
    xdt_r = ring([128, 64], BF16, "xdt", 10)
    xdw_r = ring([128, 64], BF16, "xdw", 10)
    yf_r = ring([128, 128], F32, "yf", 3)
    yt_r = ring([128, 64], F32, "yt", 4)
    sz_r = ring([128, 128], F32, "sz", 3)
    vb_r = ring([128, 128], F32, "vb", 3)
    junk = k.carve(arena, [128, 128], F32)
    ss_r = ring([128, 1], F32, "ss", 3)
    toks = []

    fwd_order = list(range(NCH))
    bwd_order = [1, 0] + list(range(NCH - 1, 1, -1))
    steps = []
    for idx in range(NCH):
        steps.append((0, fwd_order[idx]))
        steps.append((1, bwd_order[idx]))
    seen_chunk = set()

    def front(dr, c):
        lastc = 127 if dr == 0 else 0
        second = c in seen_chunk
        seen_chunk.add(c)
        want_y = need_ctx or c >= 2
        sl = slice(c * 128, (c + 1) * 128)
        st = {"dr": dr, "c": c, "second": second, "want_y": want_y, "sl": sl, "heads": []}
        pt = ptr.next()
        k.tr(pt[:, 0:128], pt.r, fm[0][:, sl], fm[0].r, ident[:], ident.r, fresh=True)
        k.tr(pt[:, 128:256], pt.r, fm[1][:, sl], fm[1].r, ident[:], ident.r, fresh=False)
        xtk, btk = xtk_r.next(), btk_r.next()
        k.copy(xtk[:], xtk.r, pt[:, 0:128], pt.r, eng="act")
        k.copy(btk[:], btk.r, pt[:, 128:256], pt.r, eng="dve")
        st["xtk"], st["btk"] = xtk, btk
        if want_y:
            pcb = psr.next()
            k.mm(pcb[:, 0:128], pcb.r, fm[1][:, sl], fm[1].r, fm[2][:, sl], fm[2].r)
            cbm = cbm_r.next()
            k.tt(cbm[:], cbm.r, pcb[:, 0:128], pcb.r, tri[:, dr, :], tri.r, ALU.mult)
        for hh in range(2):
            col = dr * 2 + hh
            hd = {"col": col}
            if want_y:
                dab = dab_r.next()
                k.copy(dab[:], dab.r, dA[:, c, col:col + 1].broadcast_to([128, 128]), dA.r, eng="pool")
                pA = psr.next()
                k.mm(pA[:, 0:128], pA.r, dab[:], dab.r, tri[:, dr, :], tri.r)
            xdw = xdw_r.next()
            k.ts(xdw[:], xdw.r, xtk[:, hh * 64:(hh + 1) * 64], xtk.r, DTW[:, c, col:col + 1], ALU.mult, reads=[DTW.r])
            hd["xdw"] = xdw
            if want_y:
                dd = d_r.next()
                k.ts(dd[:], dd.r, pA[:, 0:128], pA.r, ACOL[:, c, col:col + 1], ALU.subtract, 0.0, ALU.min, reads=[ACOL.r])
                k.act(dd[:], dd.r, dd[:], dd.r, AF.Exp)
                mt = mt_r.next()
                k.tt(mt[:], mt.r, dd[:], dd.r, cbm[:], cbm.r, ALU.mult)
                e2 = e2_r.next()
                k.act(e2[:], e2.r, pA[:, 0:128], pA.r, AF.Exp)
                ce = ce_r.next()
                k.tt(ce[:], ce.r, fm[2][:, sl], fm[2].r, e2[:], e2.r, ALU.mult)
                xdt = xdt_r.next()
                k.ts(xdt[:], xdt.r, xtk[:, hh * 64:(hh + 1) * 64], xtk.r, dtv[:, c, col:col + 1], ALU.mult, reads=[dtv.r])
                hd["mt"], hd["ce"], hd["xdt"] = mt, ce, xdt
            st["heads"].append(hd)
        return st

    def back(st):
        c, sl, second, want_y = st["c"], st["sl"], st["second"], st["want_y"]
        xtk, btk = st["xtk"], st["btk"]
        yf = yf_r.next() if (want_y and second) else None
        for hh, hd in enumerate(st["heads"]):
            col, xdw = hd["col"], hd["xdw"]
            if want_y:
                py = psr.next()
                k.mm(py[:, 0:64], py.r, hd["mt"][:], hd["mt"].r, hd["xdt"][:], hd["xdt"].r, start=True, stop=False)
                k.mm(py[:, 0:64], py.r, hd["ce"][:], hd["ce"].r, Hb[col][:], Hb[col].r, start=False, stop=True)
                if not second:
                    k.copy(yacc[:, c, hh * 64:(hh + 1) * 64], yacc.r, py[:, 0:64], py.r, eng="act")
                else:
                    yt = yt_r.next()
                    k.tt(yt[:], yt.r, py[:, 0:64], py.r, yacc[:, c, hh * 64:(hh + 1) * 64], yacc.r, ALU.add)
                    k.stt(yf[:, hh * 64:(hh + 1) * 64], yf.r, xtk[:, hh * 64:(hh + 1) * 64], xtk.r, dsk[:, hh:hh + 1],
                          yt[:], yt.r, ALU.mult, ALU.add, reads=[dsk.r])
            pst = psr.next()
            k.mm(pst[:, 0:64], pst.r, btk[:], btk.r, xdw[:], xdw.r)
            k.stt(H[col][:], H[col].r, H[col][:], H[col].r, CDEC[:, c, col:col + 1], pst[:, 0:64], pst.r, ALU.mult, ALU.add, reads=[CDEC.r])
            k.copy(Hb[col][:], Hb[col].r, H[col][:], H[col].r, eng="act")
        if want_y and second:
            vb = vb_r.next()
            k.tt(vb[:], vb.r, yf[:], yf.r, zt[:, c, :], zt.r, ALU.mult)
            ss = ss_r.next()
            k.act(junk[:], junk.r, vb[:], vb.r, AF.Square, accum_out=ss[:, 0:1], writes=[ss.r])
            toks.append(k.store(outv[sl, :], vb[:], vb.r, dst_r=k.outr("v")))
            toks.append(k.store(outs[sl, :], ss[:], ss.r, dst_r=k.outr("ssq"), slow=k.fused))

    LAG = 2
    pend = []
    for dr, c in steps:
        pend.append(front(dr, c))
        if len(pend) > LAG:
            back(pend.pop(0))
    while pend:
        back(pend.pop(0))
    if debug:
        d1 = k.dout("dbg_dt", [128, NCH * 4])
        d2 = k.dout("dbg_fm", [3, 128, 1024])
        d3 = k.dout("dbg_H", [4, 128, 64])
        d4 = k.dout("dbg_dA", [128, NCH * 4])
        d5 = k.dout("dbg_z", [128, NCH * 128])
        toks.append(k.store(d1, dtv[:].rearrange("p a b -> p (a b)"), dtv.r))
        toks.append(k.store(d4, dA[:].rearrange("p a b -> p (a b)"), dA.r))
        toks.append(k.store(d5, zt[:].rearrange("p a b -> p (a b)"), zt.r))
        for j in range(3):
            toks.append(k.store(d2[j], fm[j][:, 0:1024], fm[j].r, q="pool"))
        for j in range(4):
            toks.append(k.store(d3[j], H[j][:], H[j].r))
    return k.done(toks)


def ssd_consts():
    kk = np.arange(128)
    trif = (kk[:, None] <= kk[None, :]).astype(np.float32)
    trib = (kk[:, None] >= kk[None, :]).astype(np.float32)
    return {"ident": np.eye(128, dtype=np.float32), "tri": np.stack([trif, trib], 0)}


def ssd_inputs(l, i, xT, modp, w_in, conv_w, conv_b, dt_bias, a_log, d_skip, consts):
    g = i // 4
    w = w_in[l]
    xcols = np.arange(O_XBC + 128 * i, O_XBC + 128 * (i + 1))
    bcols = np.arange(O_XBC + 1024 + 128 * g, O_XBC + 1024 + 128 * (g + 1))
    ccols = np.arange(O_XBC + 1280 + 128 * g, O_XBC + 1280 + 128 * (g + 1))
    wfm = np.ascontiguousarray(w[:, np.concatenate([xcols, bcols, ccols])])
    h0, h1 = 2 * i, 2 * i + 1
    dtc = np.array([O_DT + h0, O_DT + h1, O_DT + 16 + h0, O_DT + 16 + h1])
    wtm = np.ascontiguousarray(np.concatenate([w[:, O_Z + 128 * i:O_Z + 128 * (i + 1)], w[:, dtc]], axis=1))
    chans = np.stack([xcols, bcols, ccols], 0) - O_XBC
    cw = np.ascontiguousarray(conv_w[l][chans].transpose(1, 0, 2))
    cb = np.ascontiguousarray(conv_b[l][chans].T)
    d = {"xT": xT, "modp": modp, "wfm": wfm, "wtm": wtm, "cw": cw, "cb": cb,
         "dtb": np.ascontiguousarray(dt_bias[l].reshape(-1)[[h0, h1, 16 + h0, 16 + h1]]),
         "alog": np.ascontiguousarray(a_log[l].reshape(-1)[[h0, h1, 16 + h0, 16 + h1]]),
         "dsk": np.ascontiguousarray(d_skip[l][[h0, h1]])}
    d.update(consts)
    return d


U32 = mybir.dt.uint32
NEXP = 16384
MIXW = 264


def build_ffn(NT, ctx_tile, k=None, pfx="", fio=None, layer=0):
    k = k or KB()
    k.pfx = pfx
    NTL = NT // 128
    if fio is None:
        xres = k.din("xres", [NT, D])
        xres_r = None
        mixT = k.din("mixT", [D, NT])
        ssq8 = k.din("ssq8", [NT, 8])
        vecs = k.din("vecs", [2, 8, D])
    else:
        xres, xres_r = fio["xres"]
        lnv = k.din("lnv", [4, D])
        mixg3 = fio["mixg"][0].rearrange("(r t c) -> r t c", r=NCORE, c=MIXW)
        wcache = fio.setdefault("wcache", {})

        def mix_win(e, kind):
            key = (id(e), kind)
            if key not in wcache:
                if kind == "lat":
                    base = e.snap(k.P.pid(e) * 1024 + CTX)
                    wcache[key] = mixg3[:, bass.ds(base, 1024), :]
                else:
                    base = e.snap(k.P.pid(e) * 32)
                    wcache[key] = mixg3[:, bass.ds(base, 32), :]
            return wcache[key]
        mixg_r = fio["mixg"][1]
        mg = fio["modg"].rearrange("(r l v n d) -> r l v n d", r=NCORE, l=DEPTH, v=2, n=6)
    normw = k.din("normw", [128, 8])
    woutd = k.din("wout", [D, D])
    wqd = k.din("wq", [D, D])
    k1Td = k.din("k1T", [128, 128])
    k2Td = k.din("k2T", [128, 128])
    uTd = k.din("uT", [D, NEXP])
    vd = k.din("v", [NEXP, D])
    identd = k.din("ident", [128, 128])
    iotad = k.din("iota", [128, 128])
    xo = k.dout("xo", [NT, D]) if fio is None else None
    x1s = k.dscratch("x1s", [NT, D])
    Wd = k.dscratch("Wd", [128, 128, NT], BF16)
    x1s_r, Wd_r = Region("x1s", multi=True), Region("Wd", multi=True)

    hpT = k.sb([128, KC, NT], BF16, "hpT")
    AX_ = k.sb([128, 16384], F32, "arenaX")
    AY = k.sb([128, max(NTL, 9) * 2048], F32, "arenaY")
    AZ = k.sb([128, 7200], F32, "arenaZ")
    identb = k.sb([128, 128], BF16, "identb")
    identf = k.sb([128, 128], F32, "identf")
    iota = k.sb([128, 128], F32, "iota")
    k1T = k.sb([128, 128], F32)
    k2T = k.sb([128, 128], F32)
    nw = k.sb([128, 8], F32)
    k.load(identb[:], identb.r, identd, q="pool")
    k.load(identf[:], identf.r, identd)
    k.load(iota[:], iota.r, iotad)
    k.load(k1T[:], k1T.r, k1Td)
    k.load(k2T[:], k2T.r, k2Td)
    k.load(nw[:], nw.r, normw)
    psr = Ring([k.ps() for _ in range(6)])
    ptr = Ring([k.ps([128, 1024], BF16, "pt") for _ in range(2)])

    def ln_stats(src, small):
        st = small["st"]
        for j in range(4):
            k.P.op("dve", lambda e, j=j: e.bn_stats(out=st[:, j * 6:(j + 1) * 6], in_=src[:, j * 512:(j + 1) * 512]),
                   reads=[src.r], writes=[st.r])
        mv = small["mv"]
        k.P.op("dve", lambda e: e.bn_aggr(out=mv[:, 0:2], in_=st[:, 0:24]), reads=[st.r], writes=[mv.r])
        k.ts(mv[:, 2:3], mv.r, mv[:, 1:2], mv.r, LN_EPS, ALU.add)
        k.act(mv[:, 2:3], mv.r, mv[:, 2:3], mv.r, AF.Sqrt)
        k.P.op("dve", lambda e: e.reciprocal(out=mv[:, 3:4], in_=mv[:, 2:3]), reads=[mv.r], writes=[mv.r])
        return mv

    wo = k.carve(AX_, [128, KC, 2048], BF16)
    k.load(wo[:], wo.r, woutd.rearrange("(c p) n -> p c n", p=128), q="pool")
    for c in range(8):
        k.ts(wo[:, c, :], wo.r, wo[:, c, :], wo.r, nw[:, c:c + 1], ALU.mult, reads=[nw.r])
    bv = [k.carve(AY, [128, 2048], F32) for _ in range(5)]
    BV_IDX = (0, 4, 5, 1, 2)

    MODN = {0: 2, 1: 4, 2: 3, 3: 5}

    def load_vec(dst, si, vi):
        if fio is None:
            k.load(dst[:], dst.r, vecs[si, vi, :].partition_broadcast(128))
        elif vi >= 4:
            k.load(dst[:], dst.r, lnv[vi - 4, :].partition_broadcast(128))
        else:
            k.load(dst[:, :].rearrange("p (r d) -> p r d", d=256), dst.r,
                   mg[:, layer, si, MODN[vi], :].partition_broadcast(128), src_r=k.outr("modg"))

    def load_bvecs(si):
        for j, vi in enumerate(BV_IDX):
            load_vec(bv[j], si, vi)
        k.ts(bv[3][:], bv[3].r, bv[3][:], bv[3].r, 1.0, ALU.add)
    xr_r = Ring([k.carve(AY, [128, 2048], F32) for _ in range(2)])
    vt_r = Ring([k.carve(AY, [128, 2048], F32) for _ in range(2)])
    mb_r = Ring([k.carve(AZ, [128, KC, 128], BF16) for _ in range(2)])
    if fio is not None:
        mt_r = Ring([k.carve(AZ, [128, NCORE, MIXW], F32) for _ in range(1)])
    hb_r = Ring([k.carve(AZ, [128, 2048], BF16) for _ in range(2)])
    smallA = Ring([{"st": k.carve(AZ, [128, 24], F32), "mv": k.carve(AZ, [128, 4], F32),
                    "sq": k.carve(AZ, [128, 8], F32), "rr": k.carve(AZ, [128, 4], F32)} for _ in range(2)])
    cur_set = None
    for t in range(NTL):
        si = 1 if t == ctx_tile else 0
        if si != cur_set:
            load_bvecs(si)
            cur_set = si
        tsl = slice(t * 128, (t + 1) * 128)
        mb = mb_r.next()
        xr = xr_r.next()
        k.load(xr[:], xr.r, xres[tsl, :], src_r=xres_r)
        sm = smallA.next()
        sq, rr = sm["sq"], sm["rr"]
        if fio is None:
            k.load(mb[:], mb.r, mixT[:, tsl].rearrange("(c p) t -> p c t", p=128), q="pool")
            k.load(sq[:], sq.r, ssq8[tsl, :])
        else:
            mt = mt_r.next()
            if t == ctx_tile:
                k.memset(mt[:], mt.r, 0.0, eng="pool")
                k.P.dma("sp", lambda e, mt=mt: e.dma_start(
                    out=mt[0:32, :, :], in_=mix_win(e, "ctx").rearrange("r t c -> t r c")),
                    mt.r, reads=[mixg_r], writes=[mt.r])
            else:
                k.P.dma("sp", lambda e, mt=mt, t=t: e.dma_start(
                    out=mt[:, :, :], in_=mix_win(e, "lat")[:, t * 128:(t + 1) * 128, :].rearrange("r t c -> t r c")),
                    mt.r, reads=[mixg_r], writes=[mt.r])
            for q4 in range(4):
                p = psr.next()
                for jj in range(4):
                    c = q4 * 4 + jj
                    src = mt[:, c, 0:128] if c < 8 else mt[:, c - 8, 128:256]
                    k.tr(p[:, jj * 128:(jj + 1) * 128], p.r, src, mt.r, identf[:], identf.r, fresh=(jj == 0))
                k.copy(mb[:, q4 * 4:(q4 + 1) * 4, :], mb.r, p[:, :].rearrange("p (c t) -> p c t", t=128), p.r,
                       eng=("act" if q4 % 2 else "dve"))
            k.copy(sq[:], sq.r, mt[:, :, 256], mt.r, eng="pool")
        k.P.op("dve", lambda e, rr=rr, sq=sq: e.reduce_sum(out=rr[:, 0:1], in_=sq[:], axis=AX.X), reads=[sq.r], writes=[rr.r])
        k.ts(rr[:, 1:2], rr.r, rr[:, 0:1], rr.r, 1.0 / 1024.0, ALU.mult, LN_EPS, ALU.add)
        k.act(rr[:, 1:2], rr.r, rr[:, 1:2], rr.r, AF.Sqrt)
        k.P.op("dve", lambda e, rr=rr: e.reciprocal(out=rr[:, 2:3], in_=rr[:, 1:2]), reads=[rr.r], writes=[rr.r])
        vt = vt_r.next()
        for nb in range(4):
            ns = slice(nb * 512, (nb + 1) * 512)
            pa, pb = psr.next(), psr.next()
            for c in range(8):
                k.mm(pa[:, :], pa.r, mb[:, c, :], mb.r, wo[:, c, ns], wo.r, start=(c == 0), stop=(c == 7))
            for c in range(8, 16):
                k.mm(pb[:, :], pb.r, mb[:, c, :], mb.r, wo[:, c, ns], wo.r, start=(c == 8), stop=(c == 15))
            k.act(vt[:, ns], vt.r, pa[:, :], pa.r, AF.Copy, scale=rr[:, 2:3], reads=[rr.r])
            k.tt(vt[:, ns], vt.r, vt[:, ns], vt.r, pb[:, :], pb.r, ALU.add)
            k.tt(vt[:, ns], vt.r, vt[:, ns], vt.r, bv[0][:, ns], bv[0].r, ALU.mult)
            k.stt(vt[:, ns], vt.r, xr[:, ns], xr.r, ALPHA, vt[:, ns], vt.r, ALU.mult, ALU.add)
        mv = ln_stats(vt, sm)
        k.ts(vt[:], vt.r, vt[:], vt.r, mv[:, 0:1], ALU.subtract, mv[:, 3:4], ALU.mult, reads=[mv.r])
        k.tt(vt[:], vt.r, vt[:], vt.r, bv[1][:], bv[1].r, ALU.mult)
        k.tt(vt[:], vt.r, vt[:], vt.r, bv[2][:], bv[2].r, ALU.add, eng="pool")
        k.store(x1s[tsl, :], vt[:], vt.r, dst_r=x1s_r)
        k.tt(xr[:], xr.r, vt[:], vt.r, bv[3][:], bv[3].r, ALU.mult)
        hb = hb_r.next()
        k.tt(hb[:], hb.r, xr[:], xr.r, bv[4][:], bv[4].r, ALU.add, eng="pool")
        for half in range(2):
            pt = ptr.next()
            for j in range(8):
                c = half * 8 + j
                k.tr(pt[:, j * 128:(j + 1) * 128], pt.r, hb[:, c * 128:(c + 1) * 128], hb.r, identb[:], identb.r, fresh=(j == 0))
            k.copy(hpT[:, half * 8:(half + 1) * 8, tsl], hpT.r, pt[:, :].rearrange("p (c t) -> p c t", t=128), pt.r,
                   eng=("act" if half else "dve"))

    k.reset(AX_); k.reset(AY); k.reset(AZ)
    wq = k.carve(AX_, [128, KC, 2048], BF16)
    k.load(wq[:], wq.r, wqd.rearrange("(c p) n -> p c n", p=128), q="pool")
    qT = k.carve(AZ, [128, 16, 128], F32)
    S1 = k.carve(AY, [128, 8, 128], F32)
    S2 = k.carve(AY, [128, 8, 128], F32)
    tmp8 = k.carve(AY, [128, 8, 256], F32)
    V1 = k.carve(AY, [128, 8, 16], F32)
    V2 = k.carve(AY, [128, 8, 16], F32)
    I1 = k.carve(AY, [128, 8, 16], U32)
    I2 = k.carve(AY, [128, 8, 16], U32)
    I1f = k.carve(AY, [128, 8, 16], F32)
    I2f = k.carve(AY, [128, 8, 16], F32)
    cand = k.carve(AZ, [128, 8, 256], F32)
    T = k.carve(AY, [128, 8, 16], F32)
    POS = k.carve(AY, [128, 8, 16], U32)
    PA = k.carve(AY, [128, 8, 16], U32)
    PB = k.carve(AY, [128, 8, 16], U32)
    PAf = k.carve(AY, [128, 8, 16], F32)
    PBf = k.carve(AY, [128, 8, 16], F32)
    OH = k.carve(AY, [128, 8 * 16, 16], F32)
    negm = k.carve(AY, [128, 8], F32)
    Zs = k.carve(AY, [128, 8], F32)
    SL = k.carve(AY, [128, 3, 128], F32)
    SLT = k.carve(AY, [128, 3, 128], F32)
    WTb = k.carve(AY, [128, 128, 128], BF16)
    oh_r = Ring([(k.carve(AZ, [128, 128], BF16), k.carve(AZ, [128, 128], BF16)) for _ in range(4)])
    iota16 = iota[:, 0:16]

    for t in range(NTL):
        tsl = slice(t * 128, (t + 1) * 128)
        for j in range(16):
            p = psr.next()
            for c in range(KC):
                k.mm(p[:, 0:128], p.r, wq[:, c, j * 128:(j + 1) * 128], wq.r, hpT[:, c, tsl], hpT.r, start=(c == 0), stop=(c == KC - 1))
            k.copy(qT[:, j, :], qT.r, p[:, 0:128], p.r, eng=("act" if j % 2 else "dve"))
        for (Sx, kT_, half) in ((S1, k1T, 0), (S2, k2T, 1)):
            for hg in range(2):
                p = psr.next()
                for hh in range(4):
                    h = hg * 4 + hh
                    k.mm(p[:, hh * 128:(hh + 1) * 128], p.r, qT[:, 2 * h + half, :], qT.r, kT_[:], kT_.r, start=True, stop=True)
                k.copy(Sx[:, hg * 4:(hg + 1) * 4, :], Sx.r, p[:, :].rearrange("p (a b) -> p a b", b=128), p.r, eng="act")
        for (Sx, Vx, Ix) in ((S1, V1, I1), (S2, V2, I2)):
            for h in range(8):
                k.P.op("dve", lambda e, h=h, Sx=Sx, Vx=Vx: e.max(out=Vx[:, h, 0:8], in_=Sx[:, h, :]), reads=[Sx.r], writes=[Vx.r])
            for h in range(8):
                k.P.op("dve", lambda e, h=h, Sx=Sx, Vx=Vx: e.match_replace(out=tmp8[:, h, 0:128], in_to_replace=Vx[:, h, 0:8], in_values=Sx[:, h, :], imm_value=-1e30),
                       reads=[Sx.r], sreads=[Vx.r], writes=[tmp8.r])
            for h in range(8):
                k.P.op("dve", lambda e, h=h, Vx=Vx: e.max(out=Vx[:, h, 8:16], in_=tmp8[:, h, 0:128]), reads=[tmp8.r], writes=[Vx.r])
            for h in range(8):
                k.P.op("dve", lambda e, h=h, Sx=Sx, Vx=Vx, Ix=Ix: e.max_index(out=Ix[:, h, 0:8], in_max=Vx[:, h, 0:8], in_values=Sx[:, h, :]),
                       reads=[Sx.r], sreads=[Vx.r], writes=[Ix.r])
            for h in range(8):
                k.P.op("dve", lambda e, h=h, Sx=Sx, Vx=Vx, Ix=Ix: e.max_index(out=Ix[:, h, 8:16], in_max=Vx[:, h, 8:16], in_values=Sx[:, h, :]),
                       reads=[Sx.r], sreads=[Vx.r], writes=[Ix.r])
        k.copy(I1f[:], I1f.r, I1[:], I1.r)
        k.copy(I2f[:], I2f.r, I2[:], I2.r)
        k.tt(cand[:].rearrange("p h (a b) -> p h a b", b=16), cand.r,
             V1[:].unsqueeze(3).broadcast_to([128, 8, 16, 16]), V1.r,
             V2[:].unsqueeze(2).broadcast_to([128, 8, 16, 16]), V2.r, ALU.add)
        for h in range(8):
            k.P.op("dve", lambda e, h=h: e.max(out=T[:, h, 0:8], in_=cand[:, h, :]), reads=[cand.r], writes=[T.r])
        for h in range(8):
            k.P.op("dve", lambda e, h=h: e.match_replace(out=tmp8[:, h, :], in_to_replace=T[:, h, 0:8], in_values=cand[:, h, :], imm_value=-1e30),
                   reads=[cand.r], sreads=[T.r], writes=[tmp8.r])
        for h in range(8):
            k.P.op("dve", lambda e, h=h: e.max(out=T[:, h, 8:16], in_=tmp8[:, h, :]), reads=[tmp8.r], writes=[T.r])
        for h in range(8):
            k.P.op("dve", lambda e, h=h: e.max_index(out=POS[:, h, 0:8], in_max=T[:, h, 0:8], in_values=cand[:, h, :]),
                   reads=[cand.r], sreads=[T.r], writes=[POS.r])
        for h in range(8):
            k.P.op("dve", lambda e, h=h: e.max_index(out=POS[:, h, 8:16], in_max=T[:, h, 8:16], in_values=cand[:, h, :]),
                   reads=[cand.r], sreads=[T.r], writes=[POS.r])
        k.ts(negm[:], negm.r, T[:, :, 0], T.r, -1.0, ALU.mult)
        for h in range(8):
            k.act(SL[:, 2, h * 16:(h + 1) * 16], SL.r, T[:, h, :], T.r, AF.Exp, bias=negm[:, h:h + 1], accum_out=Zs[:, h:h + 1],
                  reads=[negm.r], writes=[Zs.r])
        k.P.op("dve", lambda e: e.reciprocal(out=Zs[:], in_=Zs[:]), reads=[Zs.r], writes=[Zs.r])
        k.tt(SL[:, 2, :].rearrange("p (h a) -> p h a", a=16), SL.r, SL[:, 2, :].rearrange("p (h a) -> p h a", a=16), SL.r,
             Zs[:].unsqueeze(2).broadcast_to([128, 8, 16]), Zs.r, ALU.mult)
        k.ts(PA[:], PA.r, POS[:], POS.r, 4, ALU.logical_shift_right)
        k.ts(PB[:], PB.r, POS[:], POS.r, 15, ALU.bitwise_and)
        k.copy(PAf[:], PAf.r, PA[:], PA.r)
        k.copy(PBf[:], PBf.r, PB[:], PB.r)
        for (Pf, If, row) in ((PAf, I1f, 0), (PBf, I2f, 1)):
            oh4 = OH[:].rearrange("p (h a) b -> p h a b", a=16)
            k.tt(oh4, OH.r, iota16.unsqueeze(1).unsqueeze(1).broadcast_to([128, 8, 16, 16]), iota.r,
                 Pf[:].unsqueeze(3).broadcast_to([128, 8, 16, 16]), Pf.r, ALU.is_equal)
            k.tt(oh4, OH.r, oh4, OH.r, If[:].unsqueeze(2).broadcast_to([128, 8, 16, 16]), If.r, ALU.mult)
            k.P.op("dve", lambda e, row=row: e.reduce_sum(out=SL[:, row, :], in_=OH[:], axis=AX.X), reads=[OH.r], writes=[SL.r])
        p = psr.next()
        for row in range(3):
            k.tr(p[:, row * 128:(row + 1) * 128], p.r, SL[:, row, :], SL.r, identf[:], identf.r, fresh=(row == 0))
        k.copy(SLT[:], SLT.r, p[:, 0:384].rearrange("p (a b) -> p a b", b=128), p.r, eng="act")
        for g4 in range(32):
            p = psr.next()
            for j in range(4):
                tk = g4 * 4 + j
                oh2, goh1 = oh_r.next()
                k.ts(oh2[:], oh2.r, iota[:], iota.r, SLT[:, 1, tk:tk + 1], ALU.is_equal, reads=[SLT.r])
                k.ts(goh1[:], goh1.r, iota[:], iota.r, SLT[:, 0, tk:tk + 1], ALU.is_equal, SLT[:, 2, tk:tk + 1], ALU.mult, reads=[SLT.r])
                k.mm(p[:, j * 128:(j + 1) * 128], p.r, oh2[:], oh2.r, goh1[:], goh1.r, start=True, stop=True)
            k.copy(WTb[:, :, g4 * 4:(g4 + 1) * 4], WTb.r,
                   p[:, :].rearrange("p (t i) -> p i t", i=128), p.r, eng=("act" if g4 % 3 else "dve"))
        k.store(Wd[:, :, tsl].rearrange("a b t -> b a t"), WTb[:], WTb.r, dst_r=Wd_r)

    k.reset(AX_); k.reset(AY); k.reset(AZ)
    acc = k.carve(AY, [128, NTL, 2048], F32)
    k.memset(acc[:], acc.r, 0.0, eng="pool")
    G = 4
    ug_r = Ring([k.carve(AX_, [128, KC, G * 128], BF16) for _ in range(2)])
    vg_r = Ring([k.carve(AX_, [128, G, 2048], BF16) for _ in range(2)])
    wt_r = Ring([k.carve(AZ, [128, NT], BF16) for _ in range(2)])
    ga_r = Ring([k.carve(AZ, [128, NT], BF16) for _ in range(2)])
    wg_r = Ring([k.carve(AZ, [128, G, NT], BF16) for _ in range(2)])
    TB = 384 if NT % 384 == 0 else 512
    for g in range(128 // G):
        ug, vg, wg = ug_r.next(), vg_r.next(), wg_r.next()
        k.load(ug[:], ug.r, uTd[:, g * G * 128:(g + 1) * G * 128].rearrange("(c p) e -> p c e", p=128), q="pool")
        k.load(vg[:], vg.r, vd[g * G * 128:(g + 1) * G * 128, :].rearrange("(j p) n -> p j n", p=128), q="pool")
        for j in range(G):
            i1 = g * G + j
            wt = wt_r.next()
            k.load(wt[:], wt.r, Wd[i1, :, :], src_r=Wd_r)
            ga = ga_r.next()
            for tb in range(NT // TB):
                bs = slice(tb * TB, (tb + 1) * TB)
                p = psr.next()
                for c in range(KC):
                    k.mm(p[:, :TB], p.r, ug[:, c, j * 128:(j + 1) * 128], ug.r, hpT[:, c, bs], hpT.r, start=(c == 0), stop=(c == KC - 1))
                k.act(ga[:, bs], ga.r, p[:, :TB], p.r, AF.Gelu)
            k.tt(wg[:, j, :], wg.r, ga[:], ga.r, wt[:], wt.r, ALU.mult)
        for t in range(NTL):
            tsl = slice(t * 128, (t + 1) * 128)
            for nb in range(4):
                ns = slice(nb * 512, (nb + 1) * 512)
                p = psr.next()
                for j in range(G):
                    k.mm(p[:, :], p.r, wg[:, j, tsl], wg.r, vg[:, j, ns], vg.r, start=(j == 0), stop=(j == G - 1))
                k.tt(acc[:, t, ns], acc.r, acc[:, t, ns], acc.r, p[:, :], p.r, ALU.add)

    k.reset(AX_); k.reset(AZ)
    bv2 = [k.carve(AX_, [128, 2048], F32) for _ in range(3)]
    x1_r = Ring([k.carve(AX_, [128, 2048], F32) for _ in range(2)])
    ot_r = Ring([k.carve(AX_, [128, 2048], F32) for _ in range(2)])
    smallD = Ring([{"st": k.carve(AZ, [128, 24], F32), "mv": k.carve(AZ, [128, 4], F32)} for _ in range(2)])
    cur_set = None
    toks = []
    for t in range(NTL):
        si = 1 if t == ctx_tile else 0
        if si != cur_set:
            for j, vi in enumerate((3, 6, 7)):
                load_vec(bv2[j], si, vi)
            cur_set = si
        tsl = slice(t * 128, (t + 1) * 128)
        x1 = x1_r.next()
        k.load(x1[:], x1.r, x1s[tsl, :], src_r=x1s_r)
        ot = ot_r.next()
        k.tt(ot[:], ot.r, acc[:, t, :], acc.r, bv2[0][:], bv2[0].r, ALU.mult)
        k.stt(ot[:], ot.r, x1[:], x1.r, ALPHA, ot[:], ot.r, ALU.mult, ALU.add)
        sm = smallD.next()
        mv = ln_stats(ot, sm)
        k.ts(ot[:], ot.r, ot[:], ot.r, mv[:, 0:1], ALU.subtract, mv[:, 3:4], ALU.mult, reads=[mv.r])
        k.tt(ot[:], ot.r, ot[:], ot.r, bv2[1][:], bv2[1].r, ALU.mult)
        k.tt(ot[:], ot.r, ot[:], ot.r, bv2[2][:], bv2[2].r, ALU.add, eng="pool")
        if fio is None:
            toks.append(k.store(xo[tsl, :], ot[:], ot.r))
        else:
            toks.append(fio["xo_store"](t, ot))
    return k.done(toks)


def ffn_vecs(mod_l, ln1_g, ln1_b, ln2_g, ln2_b):
    out = np.zeros((2, 8, D), np.float32)
    for si in range(2):
        m = mod_l[si]
        out[si, 0] = m[2 * D:3 * D]
        out[si, 1] = m[4 * D:5 * D]
        out[si, 2] = m[3 * D:4 * D]
        out[si, 3] = m[5 * D:6 * D]
        out[si, 4], out[si, 5], out[si, 6], out[si, 7] = ln1_g, ln1_b, ln2_g, ln2_b
    return out


NLOC = 1056


def build_fused():
    k = KB(fused=True)
    nc = k.nc

    def idram(name, n, multi=True):
        return nc.dram_tensor(name, [n], F32).ap(), Region(name, multi=multi)
    modloc, modloc_r = idram("modloc", DEPTH * 2 * MODC)
    modg, modg_r = idram("modg", NCORE * DEPTH * 2 * MODC, multi=False)
    mixloc, mixloc_r = idram("mixloc", NTOK * MIXW)
    mixg, mixg_r = idram("mixg", NCORE * NTOK * MIXW, multi=False)
    xloc, xloc_r = idram("xloc", NLOC * D)
    xg, xg_r = idram("xg", NCORE * NLOC * D, multi=False)
    hts = nc.dram_tensor("hts", [128, KC, NTOK], BF16).ap()
    hts_r = Region("hts", multi=True)
    x_in = k.din("x", [SEQ, D])
    ctx_in = k.din("ctx", [CTX, D])
    xres0 = k.din("xres0", [1152, D])
    xo_out = nc.dram_tensor("xo", [1024, D], F32, kind="ExternalOutput").ap()
    mix2 = mixloc.rearrange("(t c) -> t c", c=MIXW)
    xloc2 = xloc.rearrange("(t d) -> t d", d=D)
    xg2 = xg.rearrange("(t d) -> t d", d=D)

    k.io = {"mod": modloc.rearrange("(l v n) -> l v n", l=DEPTH, v=2)}
    k.ior = {"mod": modloc_r}
    build_mod(k=k, pfx="m_")
    k.collective_allgather(modloc, modloc_r, modg, modg_r)

    toks = []
    wcache = {}
    for l in range(DEPTH):
        need_ctx = l < DEPTH - 1
        if l == 0:
            def x_src(g):
                if g < CTX:
                    return [(ctx_in[g:g + 128, :], 0, 128, None)]
                return [(x_in[g - CTX:g - CTX + 128, :], 0, 128, None)]
        else:
            def x_src(g):
                if g < CTX:
                    out = []
                    for q in range(4):
                        core = g // 32 + q
                        out.append((xg2[core * NLOC + 1024:core * NLOC + 1056, :], 32 * q, 32, xg_r))
                    return out
                t0 = g - CTX
                core = t0 // 1024
                r0 = core * NLOC + t0 % 1024
                return [(xg2[r0:r0 + 128, :], 0, 128, xg_r)]
        k.phase_reset()
        k.io = {"o": mix2[:, 128:256]}
        k.ior = {"o": mixloc_r, "modg": modg_r}
        build_attn(need_ctx, k=k, pfx="a%d_" % l, x_src=x_src, modg=modg, layer=l, hsave=(hts, hts_r))
        k.phase_reset()
        k.io = {"v": mix2[:, 0:128], "ssq": mix2[:, 256:257]}
        k.ior = {"v": mixloc_r, "ssq": mixloc_r, "modg": modg_r}
        build_ssd(need_ctx, k=k, pfx="s%d_" % l, x_src=x_src, modg=modg, layer=l, hload=(hts, hts_r))
        k.collective_allgather(mixloc, mixloc_r, mixg, mixg_r)
        k.phase_reset()
        k.io = {}
        k.ior = {"modg": modg_r}
        if need_ctx:
            def xo_store(t, ot):
                if t < 8:
                    return k.store(xloc2[t * 128:(t + 1) * 128, :], ot[:], ot.r, dst_r=xloc_r)
                return k.store(xloc2[1024:1056, :], ot[0:32, :], ot.r, dst_r=xloc_r)
            fio = {"xres": (xres0, None), "mixg": (mixg, mixg_r), "modg": modg, "xo_store": xo_store, "wcache": wcache}
            build_ffn(1152, 8, k=k, pfx="f%d_" % l, fio=fio, layer=l)
            k.collective_allgather(xloc, xloc_r, xg, xg_r)
        else:
            def xo_store(t, ot):
                return k.store(xo_out[t * 128:(t + 1) * 128, :], ot[:], ot.r)
            fio = {"xres": (xloc2[0:1024, :], xloc_r), "mixg": (mixg, mixg_r), "modg": modg, "xo_store": xo_store, "wcache": wcache}
            toks = build_ffn(1024, None, k=k, pfx="f%d_" % l, fio=fio, layer=l)
    k.P.finish(toks)
    k.P.emit()
    return k.nc


_PROG = {}


def kernel(x, c, ctx, c_ctx, w_ada, b_ada, w_in, ssd_conv_w, ssd_conv_b, ssd_dt_bias, ssd_a_log, ssd_d,
           ssd_norm_w, attn_sink, nat_rpb, w_out, ln1_g, ln1_b, peer_wq, peer_k1, peer_k2, peer_u, peer_v,
           ln2_g, ln2_b):
    f32 = np.float32
    args = [np.asarray(a, dtype=f32) for a in (x, c, ctx, c_ctx, w_ada, b_ada, w_in, ssd_conv_w, ssd_conv_b, ssd_dt_bias,
                                               ssd_a_log, ssd_d, ssd_norm_w, attn_sink, nat_rpb, w_out, ln1_g, ln1_b,
                                               peer_wq, peer_k1, peer_k2, peer_u, peer_v, ln2_g, ln2_b)]
    (x, c, ctx, c_ctx, w_ada, b_ada, w_in, ssd_conv_w, ssd_conv_b, ssd_dt_bias, ssd_a_log, ssd_d, ssd_norm_w, attn_sink,
     nat_rpb, w_out, ln1_g, ln1_b, peer_wq, peer_k1, peer_k2, peer_u, peer_v, ln2_g, ln2_b) = args
    if "nc" not in _PROG:
        _PROG["nc"] = build_fused()
    nc = _PROG["nc"]
    xl, cx = np.ascontiguousarray(x[0]), np.ascontiguousarray(ctx[0])
    aconst = attn_consts()
    sconst = ssd_consts()
    iota = np.ascontiguousarray(np.tile(np.arange(128, dtype=f32), (128, 1)))
    cv = np.stack([c.reshape(D), c_ctx.reshape(D)], 0)
    cvl = np.ascontiguousarray(cv.reshape(2, KC, 128).transpose(2, 0, 1))
    shared = {"x": xl, "ctx": cx, "iota": iota, "m_cv": cvl}
    shared.update(aconst)
    shared.update(sconst)
    for l in range(DEPTH):
        rows = list(range(1024))
        for r in range(NCORE):
            rows += list(range(1024 + 64 * r, 1024 + 64 * (r + 1))) + list(range(1536 + 64 * r, 1536 + 64 * (r + 1)))
        pf = "f%d_" % l
        shared[pf + "lnv"] = np.ascontiguousarray(np.stack([ln1_g[l], ln1_b[l], ln2_g[l], ln2_b[l]], 0))
        shared[pf + "normw"] = np.ascontiguousarray(ssd_norm_w[l].reshape(8, 128).T)
        shared[pf + "wout"] = np.ascontiguousarray(w_out[l][np.array(rows)])
        shared[pf + "wq"] = np.ascontiguousarray(peer_wq[l])
        shared[pf + "k1T"] = np.ascontiguousarray(peer_k1[l].T)
        shared[pf + "k2T"] = np.ascontiguousarray(peer_k2[l].T)
        shared[pf + "uT"] = np.ascontiguousarray(peer_u[l].T)
        shared[pf + "v"] = np.ascontiguousarray(peer_v[l])
    maps = []
    for i in range(NCORE):
        d = dict(shared)
        cols = np.concatenate([np.arange(n * D + 256 * i, n * D + 256 * (i + 1)) for n in range(6)])
        d["m_w"] = np.ascontiguousarray(w_ada[:, :, cols])
        d["m_b"] = np.ascontiguousarray(b_ada[:, cols])
        xr = np.zeros((1152, D), f32)
        xr[:1024] = xl[1024 * i:1024 * (i + 1)]
        xr[1024:1056] = cx[32 * i:32 * (i + 1)]
        d["xres0"] = xr
        for l in range(DEPTH):
            a = attn_inputs(l, i, None, None, w_in, attn_sink, nat_rpb, {})
            for nm in ("wfm", "wtm", "rpb", "sink"):
                d["a%d_%s" % (l, nm)] = a[nm]
            sd = ssd_inputs(l, i, None, None, w_in, ssd_conv_w, ssd_conv_b, ssd_dt_bias, ssd_a_log, ssd_d, {})
            for nm in ("wfm", "wtm", "cw", "cb", "dtb", "alog", "dsk"):
                d["s%d_%s" % (l, nm)] = sd[nm]
        maps.append(d)
    res = run_spmd(nc, maps)
    out = np.concatenate([res[i]["xo"] for i in range(NCORE)], 0)
    return np.ascontiguousarray(out[None].astype(f32))
```
